# Optimizing a Trainium2 kernel written in Bass

```python
import jax, jax.numpy as jnp
from jax import lax
import numpy as np

D_MODEL = 1024
BATCH = 4
SEQ = 4096
DEPTH = 2
DEC_BATCH = 32
DEC_SEQ = 8
PAST_LEN = 8192
PAGE_SIZE = 128

F32 = jnp.float32
NSA_HEADS = 8
NSA_GROUPS = 2
HEAD_DIM = 64
NSA_WIDTH = NSA_HEADS * HEAD_DIM
CMP_LEN = 32
CMP_STRIDE = 16
SEL_LEN = 64
N_SEL = 16
N_LOCAL = 2
WINDOW = 512
Q_BLOCK = 128
FORCE_BONUS = 1e4
HGRN_HEADS = 4
HGRN_DK = 128
HGRN_DV = 128
HGRN_WIDTH = HGRN_HEADS * HGRN_DV
HGRN_CHUNK = 16
MIX_WIDTH = NSA_WIDTH + HGRN_WIDTH
D_FF = 2816
N_EXPERTS = 8
TOP_K = 2
N_DENSE = (DEPTH + 1) // 2
N_MOE = DEPTH // 2
EPS = 1e-6
NEG_INF = -1e30

Q_END = NSA_WIDTH
KV_END = Q_END + 6 * NSA_GROUPS * HEAD_DIM
GATE_END = KV_END + 3 * NSA_HEADS
HQ_END = GATE_END + HGRN_HEADS * HGRN_DK
HF_END = HQ_END + HGRN_HEADS * HGRN_DK
HI_END = HF_END + HGRN_HEADS * HGRN_DV
IN_COLS = HI_END + HGRN_HEADS * HGRN_DV

kernel_name = 'nsa_hgrn2_hybrid_decode_step'


def rms_norm(x, gain):
    xf = x.astype(F32)
    y = xf * lax.rsqrt(jnp.mean(xf * xf, axis=-1, keepdims=True) + EPS)
    return (y * gain.astype(F32)).astype(x.dtype)


def alibi_slopes():
    return 2.0 ** (-8.0 * (jnp.arange(NSA_HEADS, dtype=F32) + 1.0) / NSA_HEADS)


def project(h, w_in, q_gain, k_gain):
    B, T, _ = h.shape
    z = jnp.einsum('btd,dc->btc', h, w_in)
    q = rms_norm(z[..., :Q_END].reshape(B, T, NSA_HEADS, HEAD_DIM), q_gain)
    kv = z[..., Q_END:KV_END].reshape(B, T, 3, 2, NSA_GROUPS, HEAD_DIM)
    rows = jnp.stack([kv[:, :, 0, 0], kv[:, :, 0, 1], rms_norm(kv[:, :, 1, 0], k_gain[1]), kv[:, :, 1, 1]], axis=2)
    win = jnp.stack([rms_norm(kv[:, :, 2, 0], k_gain[2]), kv[:, :, 2, 1]], axis=2)
    gates = jax.nn.sigmoid(z[..., KV_END:GATE_END].astype(F32)).reshape(B, T, 3, NSA_HEADS)
    hz = (z[..., GATE_END:HQ_END], z[..., HQ_END:HF_END], z[..., HF_END:HI_END], z[..., HI_END:IN_COLS])
    return q, rows, win, gates, hz


def compress(k_raw, v_raw, pe, cw, k_gain):
    B, T, G, D = k_raw.shape
    r_n = CMP_LEN // CMP_STRIDE
    n_chunk = T // CMP_STRIDE
    n_cmp = n_chunk - r_n + 1
    ck = k_raw[:, :n_chunk * CMP_STRIDE].reshape(B, n_chunk, CMP_STRIDE, G, D)
    cv = v_raw[:, :n_chunk * CMP_STRIDE].reshape(B, n_chunk, CMP_STRIDE, G, D)
    kc = 0.0
    vc = 0.0
    for r in range(r_n):
        sl = slice(r * CMP_STRIDE, (r + 1) * CMP_STRIDE)
        kc = kc + jnp.einsum('bnsgd,sde->bnge', ck[:, r:r + n_cmp] + pe[0, sl, None, :], cw[0, sl])
        vc = vc + jnp.einsum('bnsgd,sde->bnge', cv[:, r:r + n_cmp] + pe[1, sl, None, :], cw[1, sl])
    return rms_norm(kc, k_gain), vc


def sel_blocks(k):
    B, T, G, D = k.shape
    nb = -(-T // SEL_LEN)
    k = jnp.pad(k, ((0, 0), (0, nb * SEL_LEN - T), (0, 0), (0, 0)))
    return k.reshape(B, nb, SEL_LEN, G, D).transpose(0, 3, 1, 2, 4)


def nsa_attend(q, q_pos, kc, vc, ksb, vsb, kw, vw, kw_pos, gates, slopes):
    B, Q, H, D = q.shape
    G = kc.shape[2]
    hpg = H // G
    qf = q.astype(F32).reshape(B, Q, G, hpg, D) * (D ** -0.5)
    m = slopes.reshape(G, hpg)
    tq = q_pos.astype(F32)
    n_cmp = kc.shape[1]
    c_start = jnp.arange(n_cmp) * CMP_STRIDE
    c_ok = (c_start + CMP_LEN - 1)[None, :] <= q_pos[:, None]
    c_dist = jnp.abs(tq[:, None] - (c_start.astype(F32) + 0.5 * (CMP_LEN - 1))[None, :])
    s = jnp.einsum('bqghd,bngd->bqghn', qf, kc.astype(F32)) - m[None, None, :, :, None] * c_dist[None, :, None, None, :]
    s = jnp.where(c_ok[None, :, None, None, :], s, NEG_INF)
    p_cmp = jax.nn.softmax(s, axis=-1) * jnp.any(c_ok, axis=-1).astype(F32)[None, :, None, None, None]
    o_cmp = jnp.einsum('bqghn,bngd->bqghd', p_cmp, vc.astype(F32))
    n_blk = ksb.shape[2]
    r_s = SEL_LEN // CMP_STRIDE
    r_c = CMP_LEN // CMP_STRIDE
    n_tap = r_s + r_c - 1
    taps = jnp.asarray(np.convolve(np.ones(r_s), np.ones(r_c)) / r_c, F32)
    imp = jnp.pad(jnp.sum(p_cmp, axis=3), ((0, 0), (0, 0), (0, 0), (r_c - 1, r_s * n_blk - n_cmp)))
    tap_idx = (jnp.arange(n_blk) * r_s)[:, None] + jnp.arange(n_tap)[None, :]
    blk_imp = jnp.einsum('bqgjk,k->bqgj', imp[..., tap_idx], taps)
    back = (q_pos // SEL_LEN)[:, None] - jnp.arange(n_blk)[None, :]
    forced = ((jnp.arange(n_blk)[None, :] == 0) | ((back >= 0) & (back < N_LOCAL))).astype(F32)
    score = jnp.where((back >= 0)[None, :, None, :], blk_imp + FORCE_BONUS * forced[None, :, None, :], -1.0)
    n_pick = min(N_SEL, n_blk)
    _, sel = lax.top_k(score, n_pick)
    gather = jax.vmap(jax.vmap(lambda kb, ix: kb[ix]))
    sel_t = sel.transpose(0, 2, 1, 3)
    kg = gather(ksb, sel_t).astype(F32)
    vg = gather(vsb, sel_t).astype(F32)
    k_pos = sel[..., None] * SEL_LEN + jnp.arange(SEL_LEN)
    s_dist = q_pos[None, :, None, None, None] - k_pos
    s = jnp.einsum('bqghd,bgqpld->bqghpl', qf, kg) - m[None, None, :, :, None, None] * jnp.abs(s_dist).astype(F32)[:, :, :, None]
    s = jnp.where((s_dist >= 0)[:, :, :, None], s, NEG_INF).reshape(B, Q, G, hpg, n_pick * SEL_LEN)
    o_sel = jnp.einsum('bqghk,bgqkd->bqghd', jax.nn.softmax(s, axis=-1), vg.reshape(B, G, Q, n_pick * SEL_LEN, D))
    w_dist = q_pos[:, None] - kw_pos[None, :]
    w_ok = (w_dist >= 0) & (w_dist < WINDOW) & (kw_pos >= 0)[None, :]
    s = jnp.einsum('bqghd,bkgd->bqghk', qf, kw.astype(F32)) - m[None, None, :, :, None] * jnp.abs(w_dist).astype(F32)[None, :, None, None, :]
    s = jnp.where(w_ok[None, :, None, None, :], s, NEG_INF)
    o_win = jnp.einsum('bqghk,bkgd->bqghd', jax.nn.softmax(s, axis=-1), vw.astype(F32))
    g = gates.astype(F32).reshape(B, Q, 3, G, hpg)[..., None]
    o = g[:, :, 0] * o_cmp + g[:, :, 1] * o_sel + g[:, :, 2] * o_win
    return o.reshape(B, Q, H * D).astype(q.dtype)


def nsa_prompt(q, rows, win, gates, kc, vc, slopes):
    B, S = q.shape[:2]
    ksb = sel_blocks(rows[:, :, 2])
    vsb = sel_blocks(rows[:, :, 3])
    win_pad = jnp.pad(win, ((0, 0), (WINDOW, 0), (0, 0), (0, 0), (0, 0)))

    def block(i):
        start = i * Q_BLOCK
        wb = lax.dynamic_slice_in_dim(win_pad, start, Q_BLOCK + WINDOW, axis=1)
        return nsa_attend(lax.dynamic_slice_in_dim(q, start, Q_BLOCK, axis=1), start + jnp.arange(Q_BLOCK),
                          kc, vc, ksb, vsb, wb[:, :, 0], wb[:, :, 1], start - WINDOW + jnp.arange(Q_BLOCK + WINDOW),
                          lax.dynamic_slice_in_dim(gates, start, Q_BLOCK, axis=1), slopes)

    out = lax.map(block, jnp.arange(S // Q_BLOCK))
    return out.transpose(1, 0, 2, 3).reshape(B, S, NSA_WIDTH)


def hgrn2_chunked(q, log_f, k, v, s0):
    B, T, H, DK = q.shape
    C = HGRN_CHUNK
    n = -(-T // C)
    pad = n * C - T
    def chunks(a):
        a = jnp.pad(a, ((0, 0), (0, pad), (0, 0), (0, 0)))
        return a.reshape(B, n, C, H, a.shape[-1]).transpose(1, 0, 3, 2, 4)
    causal = jnp.tril(jnp.ones((C, C), bool))[None, None, :, :, None]

    def step(S, inp):
        qi, gi, ki, vi = inp
        b = jnp.cumsum(gi, axis=2)
        dec = jnp.exp(jnp.where(causal, b[:, :, :, None, :] - b[:, :, None, :, :], NEG_INF))
        A = jnp.einsum('bhtc,bhsc,bhtsc->bhts', qi, ki, dec)
        o = jnp.einsum('bhts,bhsv->bhtv', A, vi) + jnp.einsum('bhtc,bhcv->bhtv', qi * jnp.exp(b), S)
        b_last = b[:, :, -1:, :]
        S_new = S * jnp.exp(b_last[:, :, 0, :, None]) + jnp.einsum('bhsc,bhsv->bhcv', ki * jnp.exp(b_last - b), vi)
        return S_new, o

    S, o = lax.scan(step, s0, (chunks(q), chunks(log_f), chunks(k), chunks(v)))
    o = o.transpose(1, 0, 3, 2, 4).reshape(B, n * C, H, v.shape[-1])[:, :T]
    return o, S


def hgrn2_mix(hz, lb, o_gain, s0):
    zq, zf, zi, zg = hz
    B, T, _ = zq.shape
    shp = (B, T, HGRN_HEADS, HGRN_DK)
    q = jax.nn.silu(zq.astype(F32)).reshape(shp)
    zf = zf.astype(F32).reshape(shp)
    lb = lb.astype(F32).reshape(HGRN_HEADS, HGRN_DK)
    log_f = jnp.logaddexp(jnp.log(lb), jnp.log1p(-lb) + jax.nn.log_sigmoid(zf))
    k = (1.0 - lb) * jax.nn.sigmoid(-zf)
    v = zi.astype(F32).reshape(B, T, HGRN_HEADS, HGRN_DV)
    o, S = hgrn2_chunked(q, log_f, k, v, s0.astype(F32))
    o = rms_norm(o, o_gain) * jax.nn.silu(zg.astype(F32)).reshape(B, T, HGRN_HEADS, HGRN_DV)
    return o.reshape(B, T, HGRN_WIDTH).astype(zq.dtype), S


def swiglu(h, wg, wu, wd):
    a = jnp.einsum('btd,df->btf', h, wg)
    b = jnp.einsum('btd,df->btf', h, wu)
    return jnp.einsum('btf,fd->btd', jax.nn.silu(a) * b, wd)


def moe_swiglu(h, router, wg, wu, wd):
    logits = jnp.einsum('btd,de->bte', h, router).astype(F32)
    top_val, top_idx = lax.top_k(logits, TOP_K)
    weights = jax.nn.softmax(top_val, axis=-1)
    gate = jnp.sum(jax.nn.one_hot(top_idx, N_EXPERTS, dtype=F32) * weights[..., None], axis=-2).astype(h.dtype)
    y = jnp.zeros_like(h)
    for e in range(N_EXPERTS):
        y = y + gate[..., e:e + 1] * swiglu(h, wg[e], wu[e], wd[e])
    return y


def setup_inputs(seed: int = 0) -> dict:
    key = jax.random.key(seed)
    ks = jax.random.split(key, 24)

    def nrm(k, shape, scale):
        return jax.random.normal(k, shape, F32) * scale

    n_pages = PAST_LEN // PAGE_SIZE
    n_used = DEC_BATCH * n_pages
    n_pool = n_used + max(1, n_used // 4)
    w_buf = min(WINDOW, PAST_LEN)
    page_table = jax.random.permutation(ks[0], n_pool)[:n_used].reshape(DEC_BATCH, n_pages).astype(jnp.int32)
    return {
        'x_prompt': nrm(ks[1], (BATCH, SEQ, D_MODEL), 1.0),
        'x_sample': nrm(ks[2], (DEC_BATCH, DEC_SEQ, D_MODEL), 1.0),
        'cache_kv': nrm(ks[3], (DEPTH, n_pool, PAGE_SIZE, 4, NSA_GROUPS, HEAD_DIM), 1.0),
        'state_win_kv': nrm(ks[4], (DEPTH, DEC_BATCH, w_buf, 2, NSA_GROUPS, HEAD_DIM), 1.0),
        'state_hgrn': nrm(ks[5], (DEPTH, DEC_BATCH, HGRN_HEADS, HGRN_DK, HGRN_DV), 0.3),
        'page_table': page_table,
        'norm_mix': 1.0 + nrm(ks[6], (DEPTH, D_MODEL), 0.02),
        'norm_ffn': 1.0 + nrm(ks[7], (DEPTH, D_MODEL), 0.02),
        'w_in': nrm(ks[8], (DEPTH, D_MODEL, IN_COLS), D_MODEL ** -0.5),
        'q_gain': 1.0 + nrm(ks[9], (DEPTH, HEAD_DIM), 0.02),
        'k_gain': 1.0 + nrm(ks[10], (DEPTH, 3, HEAD_DIM), 0.02),
        'cmp_pe': nrm(ks[11], (DEPTH, 2, CMP_LEN, HEAD_DIM), 0.1),
        'cmp_w': nrm(ks[12], (DEPTH, 2, CMP_LEN, HEAD_DIM, HEAD_DIM), (CMP_LEN * HEAD_DIM) ** -0.5),
        'hgrn_lb_logits': nrm(ks[13], (DEPTH, HGRN_HEADS * HGRN_DK), 1.0),
        'hgrn_o_gain': 1.0 + nrm(ks[14], (DEPTH, HGRN_DV), 0.02),
        'w_out': nrm(ks[15], (DEPTH, MIX_WIDTH, D_MODEL), MIX_WIDTH ** -0.5),
        'ffn_w_gate': nrm(ks[16], (N_DENSE, D_MODEL, D_FF), D_MODEL ** -0.5),
        'ffn_w_up': nrm(ks[17], (N_DENSE, D_MODEL, D_FF), D_MODEL ** -0.5),
        'ffn_w_down': nrm(ks[18], (N_DENSE, D_FF, D_MODEL), D_FF ** -0.5),
        'moe_router': nrm(ks[19], (N_MOE, D_MODEL, N_EXPERTS), D_MODEL ** -0.5),
        'moe_w_gate': nrm(ks[20], (N_MOE, N_EXPERTS, D_MODEL, D_FF), D_MODEL ** -0.5),
        'moe_w_up': nrm(ks[21], (N_MOE, N_EXPERTS, D_MODEL, D_FF), D_MODEL ** -0.5),
        'moe_w_down': nrm(ks[22], (N_MOE, N_EXPERTS, D_FF, D_MODEL), D_FF ** -0.5),
    }


def reference(x_prompt, x_sample, cache_kv, state_win_kv, state_hgrn, page_table,
              norm_mix, norm_ffn, w_in, q_gain, k_gain, cmp_pe, cmp_w, hgrn_lb_logits, hgrn_o_gain, w_out,
              ffn_w_gate, ffn_w_up, ffn_w_down, moe_router, moe_w_gate, moe_w_up, moe_w_down):
    slopes = alibi_slopes()
    sm = jax.nn.softmax(hgrn_lb_logits.astype(F32), axis=0)
    lower = jnp.concatenate([jnp.zeros_like(sm[:1]), jnp.cumsum(sm[1:], axis=0)], axis=0)
    B, S, _ = x_prompt.shape
    DB, DS, _ = x_sample.shape
    n_pages = page_table.shape[1]
    past_len = n_pages * PAGE_SIZE
    w_buf = state_win_kv.shape[2]
    xp, xs = x_prompt, x_sample
    kv_p, kv_s, win_p, win_s, hs_p, hs_s = [], [], [], [], [], []
    for l in range(DEPTH):
        if l % 2 == 0:
            i = l // 2
            def ffn(h):
                return swiglu(h, ffn_w_gate[i], ffn_w_up[i], ffn_w_down[i])
        else:
            i = l // 2
            def ffn(h):
                return moe_swiglu(h, moe_router[i], moe_w_gate[i], moe_w_up[i], moe_w_down[i])
        hp = rms_norm(xp, norm_mix[l])
        q, rows, win, gates, hz = project(hp, w_in[l], q_gain[l], k_gain[l])
        kc, vc = compress(rows[:, :, 0], rows[:, :, 1], cmp_pe[l], cmp_w[l], k_gain[l, 0])
        o_nsa = nsa_prompt(q, rows, win, gates, kc, vc, slopes)
        o_hg, s_fin = hgrn2_mix(hz, lower[l], hgrn_o_gain[l], jnp.zeros((B, HGRN_HEADS, HGRN_DK, HGRN_DV), F32))
        xp = xp + jnp.einsum('btm,md->btd', jnp.concatenate([o_nsa, o_hg], axis=-1), w_out[l])
        xp = xp + ffn(rms_norm(xp, norm_ffn[l]))
        kv_p.append(rows)
        win_p.append(win[:, -min(WINDOW, S):])
        hs_p.append(s_fin.astype(x_prompt.dtype))
        hs = rms_norm(xs, norm_mix[l])
        q, rows, win, gates, hz = project(hs, w_in[l], q_gain[l], k_gain[l])
        past = cache_kv[l][page_table].reshape(DB, past_len, 4, NSA_GROUPS, HEAD_DIM)
        rows_all = jnp.concatenate([past, rows.astype(past.dtype)], axis=1)
        kc, vc = compress(rows_all[:, :, 0], rows_all[:, :, 1], cmp_pe[l], cmp_w[l], k_gain[l, 0])
        win_all = jnp.concatenate([state_win_kv[l], win.astype(state_win_kv.dtype)], axis=1)
        o_nsa = nsa_attend(q, past_len + jnp.arange(DS), kc, vc, sel_blocks(rows_all[:, :, 2]), sel_blocks(rows_all[:, :, 3]),
                           win_all[:, :, 0], win_all[:, :, 1], past_len - w_buf + jnp.arange(w_buf + DS), gates, slopes)
        o_hg, s_new = hgrn2_mix(hz, lower[l], hgrn_o_gain[l], state_hgrn[l])
        xs = xs + jnp.einsum('btm,md->btd', jnp.concatenate([o_nsa, o_hg], axis=-1), w_out[l])
        xs = xs + ffn(rms_norm(xs, norm_ffn[l]))
        kv_s.append(rows)
        win_s.append(win_all[:, -w_buf:])
        hs_s.append(s_new.astype(state_hgrn.dtype))
    return (xp, xs, jnp.stack(kv_p), jnp.stack(kv_s), jnp.stack(win_p), jnp.stack(win_s), jnp.stack(hs_p), jnp.stack(hs_s))
```

```python
import os
import numpy as np
import ml_dtypes
from contextlib import ExitStack
import concourse.bass as bass
import concourse.mybir as mybir
from concourse.bass_utils import run_bass_kernel_spmd

F32 = mybir.dt.float32
BF16 = mybir.dt.bfloat16
I32 = mybir.dt.int32
AF = mybir.ActivationFunctionType
ALU = mybir.AluOpType
AX = mybir.AxisListType

D = 1024
KC = 8
NH = 8
G = 2
HD = 64
HH = 4
HK = 128
IN_COLS = 3352
EPS = 1e-6
NEG = -30000.0
SAME_SYNC = True
NDMA = 48
KA = 69


def full_cfg():
    return dict(S=4096, PAST=8192, NSEQ=4, DS=8, DFF=2816, NE=8, NSEL=16, NPOOL=2560, B=4, DB=32, WIN=512)


def derive(cfg):
    c = dict(cfg)
    c["NT"] = c["S"] // 128
    c["TS"] = c["PAST"] // 128
    c["NPAGE"] = c["PAST"] // 128
    c["NBP"] = c["S"] // 64
    c["NBS"] = c["PAST"] // 64 + 1
    c["NCP"] = c["S"] // 16 - 1
    c["NCS"] = c["PAST"] // 16 - 1
    c["NVT"] = (max(c["NCP"], c["NCS"]) + 127) // 128
    c["NBPAD"] = ((max(c["NBP"], c["NBS"]) + 3) // 4) * 4
    c["ND"] = max(c["NT"], c["TS"] + 1)
    c["NFC"] = c["DFF"] // 128
    c["NSR"] = c["NSEQ"] * c["DS"]
    c["ROWS"] = c["S"] + c["NSR"]
    return c


def make_consts(c):
    bf = ml_dtypes.bfloat16
    r = np.arange(128)
    K = {}
    K["identb"] = np.eye(128, dtype=np.float32).astype(bf)
    K["identf"] = np.eye(128, dtype=np.float32)
    same = (r[:, None] // 32) == (r[None, :] // 32)
    K["tri32"] = ((r[:, None] <= r[None, :]) & same).astype(np.float32)
    K["blk32"] = same.astype(np.float32)
    K["chunkind"] = ((r[:, None] // 32) == np.arange(4)[None, :]).astype(np.float32)
    j = r[:, None]; i = r[None, :]
    K["causal"] = np.where(j > i, NEG, 0.0).astype(bf)
    K["lowmask"] = np.where(j <= i, NEG, 0.0).astype(bf)
    cm = np.zeros((128, 17, 128), np.float32)
    for w in range(17):
        cm[:, w, :] = np.where(16 * j + 31 - i > 128 * w, NEG, 0.0)
    K["cmpmask"] = cm.astype(bf)
    slopes = 2.0 ** (-8.0 * (np.arange(NH) + 1.0) / NH)
    qa = np.zeros((128, NH, 128), np.float32)
    for h in range(NH):
        qa[64, h, :] = slopes[h] * 128; qa[65, h, :] = slopes[h]; qa[66, h, :] = slopes[h]
        qa[67, h, :] = -slopes[h] * 128; qa[68, h, :] = -slopes[h] * r
    K["qabase"] = qa.astype(bf)
    tc = np.ones((128, c["ND"] + 1), np.float32)
    tc[67, :] = np.arange(c["ND"] + 1)
    K["tcol"] = tc
    SC = c["S"]
    ka = np.zeros((5, SC), np.float32)
    cols = np.arange(SC)
    ka[0] = cols // 128; ka[1] = cols % 128; ka[2] = 0; ka[3] = 1; ka[4] = 1
    K["kaS"] = ka.astype(bf)
    NV = c["NVT"] * 128
    kc = np.zeros((5, NV), np.float32)
    cols = np.arange(NV)
    kc[0] = 16 * (cols // 128); kc[1] = 16 * (cols % 128); kc[2] = 15.5; kc[3] = 1; kc[4] = 1
    K["kaC"] = kc.astype(bf)
    T = np.zeros((128, c["NVT"], c["NBPAD"]), np.float32)
    taps = [0.5, 1, 1, 1, 0.5]
    for v in range(c["NVT"]):
        for rr in range(128):
            n = 128 * v + rr
            for k in range(5):
                num = n + 1 - k
                if num >= 0 and num % 4 == 0 and num // 4 < c["NBPAD"]:
                    T[rr, v, num // 4] = taps[k]
    K["Ttab"] = T.astype(bf)
    R = 192
    Vm = np.zeros((128, R), np.float32); Cm = np.zeros((128, R), np.float32)
    for ii in range(128):
        hi = 1 if ii >= 64 else 0
        for rr in range(R):
            rel = rr - 128
            valid = rel <= hi
            forced = rel in (hi, hi - 1)
            Vm[ii, rr] = 1.0 if valid else 0.0
            Cm[ii, rr] = (1e4 if forced else 0.0) + (0.0 if valid else -1.0)
    K["Vm"] = Vm; K["Cm"] = Cm
    return K


CDT = {"identb": BF16, "identf": F32, "tri32": F32, "blk32": F32, "chunkind": F32, "trimask4": F32,
       "causal": BF16, "lowmask": BF16, "cmpmask": BF16, "qabase": BF16, "tcol": F32, "kaS": BF16, "kaC": BF16,
       "Ttab": BF16, "Vm": F32, "Cm": F32}


class Tk:
    __slots__ = ("w", "r")

    def __init__(self):
        self.w = {}
        self.r = {}


class Q:
    def __init__(self, name, semi):
        self.name = name; self.semi = semi; self.count = 0; self.waited = {}; self.ops = []


class Prog:
    def __init__(self, nc, es):
        self.nc = nc; self.es = es
        self.sems = []
        self.q = {}
        for n in ("pe", "act", "dve", "pool", "sp"):
            self.q[n] = Q(n, self.new_sem("q" + n))
        self.dma_sems = [self.new_sem("d%d" % i) for i in range(NDMA)]
        self.dma_vals = [0] * NDMA
        self.dma_set = set(self.dma_sems)
        self.dma_rr = 0
        self.ninst = 0

    def new_sem(self, name):
        s = self.es.enter_context(self.nc.semaphore(name))
        self.sems.append(s)
        return len(self.sems) - 1

    def emit(self, eng, fn, r=(), w=(), dma=False):
        q = self.q[eng]
        deps = {}
        dset = self.dma_set

        def add(s, v):
            if deps.get(s, 0) < v:
                deps[s] = v
        for t in r:
            for s, v in t.w.items():
                add(s, v)
        for t in w:
            for s, v in t.w.items():
                if dma and s in dset:
                    continue
                add(s, v)
            for s, v in t.r.items():
                add(s, v)
        if dma:
            slot = self.dma_rr; self.dma_rr = (slot + 1) % NDMA
            sem = self.dma_sems[slot]; pv = self.dma_vals[slot]
            if pv > 0:
                add(sem, pv)
            self.dma_vals[slot] = pv + 16
            ev = (sem, pv + 16); inc = (sem, 16)
        else:
            q.count += 1
            ev = (q.semi, q.count); inc = (q.semi, 1)
        waits = []
        for s, v in deps.items():
            if s == q.semi and not dma and (not SAME_SYNC or eng == "pe"):
                continue
            if q.waited.get(s, 0) >= v:
                continue
            q.waited[s] = v; waits.append((s, v))
        q.ops.append((waits, fn, inc))
        self.ninst += 1 + len(waits)
        s, v = ev
        for t in r:
            if t.r.get(s, 0) < v:
                t.r[s] = v
        for t in w:
            if dma and not t.r:
                if t.w.get(s, 0) < v:
                    t.w[s] = v
            else:
                t.w = {s: v}
            t.r = {}
        return ev

    def barrier(self):
        snap = [(q.semi, q.count) for q in self.q.values() if q.count > 0]
        snap += [(self.dma_sems[i], v) for i, v in enumerate(self.dma_vals) if v > 0]
        for n, q in self.q.items():
            waits = []
            for s, v in snap:
                if s == q.semi:
                    continue
                if q.waited.get(s, 0) >= v:
                    continue
                q.waited[s] = v; waits.append((s, v))
            q.count += 1
            q.ops.append((waits, lambda e: e.nop(), (q.semi, 1)))
            self.ninst += 1 + len(waits)

    def flush(self, final=False):
        P = self
        with self.nc.Block() as block:
            def rp(name):
                def f(e):
                    q = P.q[name]
                    for waits, fn, inc in q.ops:
                        for s, v in waits:
                            e.wait_ge(P.sems[s], v)
                        fn(e).then_inc(P.sems[inc[0]], inc[1])
                    q.ops = []
                    if final and name == "sp":
                        for i, v in enumerate(P.dma_vals):
                            if v > 0:
                                e.wait_ge(P.sems[P.dma_sems[i]], v)
                        for n2, q2 in P.q.items():
                            if n2 != "sp" and q2.count > 0:
                                e.wait_ge(P.sems[q2.semi], q2.count)
                return f
            block.sync(rp("sp")); block.tensor(rp("pe")); block.scalar(rp("act")); block.vector(rp("dve")); block.gpsimd(rp("pool"))


def build_program(cfg, stop_after=99, tiles_limit=None):
    c = derive(cfg)
    S, PAST, NSEQ, DS, DFF, NE, NSEL = c["S"], c["PAST"], c["NSEQ"], c["DS"], c["DFF"], c["NE"], c["NSEL"]
    NT, TS, NPAGE = c["NT"], c["TS"], c["NPAGE"]
    NBPAD, NVT, ND, NFC, ROWS, NSR = c["NBPAD"], c["NVT"], c["ND"], c["NFC"], c["ROWS"], c["NSR"]
    WIN = c["WIN"]; NPOOL = c["NPOOL"]
    NWT = WIN // 128
    consts = make_consts(c)

    nc = bass.Bass("TRN2", target_bir_lowering=False)
    es = ExitStack()
    P = Prog(nc, es)

    def din(name, shape, dt=F32):
        return nc.dram_tensor(name, list(shape), dt, kind="ExternalInput").ap()

    def dout(name, shape, dt=F32):
        return nc.dram_tensor(name, list(shape), dt, kind="ExternalOutput").ap()

    def dint(name, shape, dt=F32):
        return nc.dram_tensor(name, list(shape), dt, kind="Internal").ap()

    xp_in = din("xp", [S, D]); xs_in = din("xs", [NSR, D])
    cache = din("cache", [2 * NPOOL * 128 * 2, 256])
    swin = din("swin", [2, NSEQ, WIN, 256]); shg = din("shg", [2, NSEQ, HH, HK, HK])
    ptab = din("ptab", [128, NSEQ * NPAGE], I32)
    NTH = NT // 2
    ridx_in = din("ridx", [128, NTH], I32)
    nmix = din("nmix", [2, 128, D]); nffn = din("nffn", [2, 128, D])
    w_in = din("w_in", [2, D, IN_COLS]); w_out = din("w_out", [2, D, D])
    qg = din("qg", [2, 128, HD]); kg = din("kg", [2, 128, 3 * HD])
    cpe = din("cpe", [2, 2, 32, HD]); cw = din("cw", [2, 2, 32, HD, HD])
    lbl = din("lbl", [128, 2, HH * HK]); ogn = din("ogn", [2, 128, HK])
    fwg = din("fwg", [D, DFF]); fwu = din("fwu", [D, DFF]); fwd = din("fwd", [DFF, D])
    rtr = din("rtr", [D, NE]); mwg = din("mwg", [NE, D, DFF]); mwu = din("mwu", [NE, D, DFF]); mwd = din("mwd", [NE, DFF, D])
    cin = {k: din("c_" + k, consts[k].shape, CDT[k]) for k in consts}

    y_p = dout("y_p", [S // 2, D]); y_s = dout("y_s", [NSR, D])
    kv_p = dout("kv_p", [2, S, 512]); kv_s = dout("kv_s", [2, NSR, 512])
    win_p = dout("win_p", [2, WIN, 256]); win_s = dout("win_s", [2, NSEQ, WIN, 256])
    hg_p = dout("hg_p", [2, HH, HK, HK]); hg_s = dout("hg_s", [2, NSEQ, HH, HK, HK])
    x1buf = dint("x1buf", [ROWS, D]); x2buf = dint("x2buf", [ROWS, D]); zst = dint("zst", [NSR, IN_COLS])

    def sbx(stack, name, shape, dt=F32):
        return stack.enter_context(nc.sbuf_tensor(name, list(shape), dt))

    def sb(name, shape, dt=F32):
        return sbx(es, name, shape, dt)

    def TK(n=None):
        return Tk() if n is None else [Tk() for _ in range(n)]

    def mm(out, lhsT, rhs, start, stop, r, w):
        P.emit("pe", lambda e: e.matmul(out, lhsT, rhs, start=start, stop=stop, skip_group_check=True), r, w)

    def tr(out, in_, ident, r, w):
        P.emit("pe", lambda e: e.transpose(out, in_, ident), r, w)

    def act(out, in_, func, r, w, bias=None, scale=None, accum=None):
        kw = {}
        if bias is not None: kw["bias"] = bias
        if scale is not None: kw["scale"] = scale
        if accum is not None: kw["accum_out"] = accum
        P.emit("act", lambda e: e.activation(out, in_, func, **kw), r, w)

    def tt(eng, out, in0, in1, op, r, w):
        P.emit(eng, lambda e: e.tensor_tensor(out, in0, in1, op), r, w)

    def ts(eng, out, in0, s1, s2, op0, op1, r, w):
        if op1 is None:
            P.emit(eng, lambda e: e.tensor_scalar(out, in0, s1, None, op0), r, w)
        else:
            P.emit(eng, lambda e: e.tensor_scalar(out, in0, s1, s2, op0, op1), r, w)

    def stt(out, in0, scalar, in1, op0, op1, r, w):
        P.emit("dve", lambda e: e.scalar_tensor_tensor(out, in0, scalar, in1, op0, op1), r, w)

    def cp(eng, out, in_, r, w):
        if eng == "act":
            P.emit("act", lambda e: e.activation(out, in_, AF.Copy), r, w)
        else:
            P.emit(eng, lambda e: e.tensor_copy(out, in_), r, w)

    def recip(out, in_, r, w):
        P.emit("dve", lambda e: e.reciprocal(out, in_), r, w)

    def memset(eng, ap, val, w):
        P.emit(eng, lambda e: e.memset(ap, val), (), w if isinstance(w, list) else [w])

    def dma(eng, out, in_, r, w, slow=False):
        if slow:
            P.emit(eng, lambda e: e.dma_start(out=out, in_=in_, allow_slow_non_contiguous=True), r, w, dma=True)
        else:
            P.emit(eng, lambda e: e.dma_start(out=out, in_=in_), r, w, dma=True)

    def gather(out, in_, idx_ap, r, w):
        P.emit("pool", lambda e: e.indirect_dma_start(out=out, out_offset=None, in_=in_,
                                                       in_offset=bass.IndirectOffsetOnAxis(ap=idx_ap, axis=0)), r, w, dma=True)

    def hv3(ap, h):
        return ap.rearrange("p (h d) -> p h d", h=h)

    C = {}; CT = {}
    for k in consts:
        if k in ("kaS", "kaC"):
            continue
        C[k] = sb("k_" + k, consts[k].shape, CDT[k]); CT[k] = TK()
        dma("sp", C[k][:], cin[k][:], [], [CT[k]])
    ones_bf = sb("ones_bf", [128, 128], BF16); t_ones = TK()
    memset("pool", ones_bf[:], 1.0, t_ones)
    epsb = sb("epsb", [128, 1]); t_eps = TK()
    memset("pool", epsb[:], EPS, t_eps)

    PSF = [es.enter_context(nc.psum_tensor("psf%d" % i, [128, 512], F32)) for i in range(6)]
    tPSF = [TK() for _ in range(6)]
    PSB = [es.enter_context(nc.psum_tensor("psb%d" % i, [128, 1024], BF16)) for i in range(2)]
    tPSB = [TK() for _ in range(2)]
    rr = {"f": 0, "b": 0, "nrot": 3}

    def psf():
        i = rr["f"] % rr["nrot"]; rr["f"] += 1
        return PSF[i], tPSF[i]

    def psb():
        i = rr["b"] % 2; rr["b"] += 1
        return PSB[i], tPSB[i]

    gnorm = sb("gnorm", [128, D]); t_gn = TK()
    qgs = sb("qgs", [128, HD]); kgs = sb("kgs", [128, 3 * HD]); t_qg = TK(); t_kg = TK()
    ogs = sb("ogs", [128, HK]); t_og = TK()
    lb = sb("lb", [128, HH * HK]); oml = sb("oml", [128, HH * HK]); t_lb = TK()
    cw_sb = sb("cw_sb", [128, 2, 32, HD], BF16); t_cw = TK()
    peT = sb("peT", [128, 2, 32], BF16); t_pe = TK()
    pe_raw = sb("pe_raw", [128, 2, 32]); t_per = TK()
    crow = sb("crow", [1, 2, HD], BF16); t_crow = TK()
    xt = sb("xt", [128, D]); t_xt = TK()
    x1t = xt; t_x1t = t_xt
    sq = sb("sq", [128, 512]); t_sq = TK()
    st1 = sb("st1", [128, 16]); t_st1 = TK()
    hn = sb("hn", [128, D], BF16); t_hn = TK()
    junk = hn; t_junk = t_hn
    hT = sb("hT", [128, KC, 128], BF16); t_hT = TK()
    pidx = sb("pidx", [128, NSEQ * NPAGE], I32); t_pidx = TK()
    pidx_v = [[sb("pidx_v%d_%d" % (l_, h_), [128, NSEQ * NPAGE], I32) for h_ in range(2)] for l_ in range(2)]
    pidxf = sb("pidxf", [128, NSEQ * NPAGE]); t_pidxf = TK()
    iota_p = sb("iota_p", [128, 1]); t_iota = TK()
    ridx = sb("ridx_sb", [128, NTH], I32); t_ridx = TK()
    W0f = sb("W0f", [128, NSEQ * NPAGE]); t_W0f = TK()

    def mixer_phase(l, sample):
        ps_ = ExitStack()
        SC = (PAST + 128) if sample else S
        NTC = SC // 128

        def sbp(name, shape, dt=F32):
            return sbx(ps_, ("s%d_" % l if sample else "p%d_" % l) + name, shape, dt)
        if not sample:
            win_sb = sbp("win_sb", [128, KC, IN_COLS], BF16); t_win = TK()
            kselT = [sbp("kselT%d" % g, [KA, SC], BF16) for g in range(G)]; t_ksel = [TK(NTC) for _ in range(G)]
            vsel = sbp("vsel", [128, NTC, G, 65], BF16); t_vsel = TK(NTC)
        else:
            zs = sbp("zs", [8, IN_COLS]); t_zs = TK()
            kpg = [[sbp("kpg%d_%d" % (g, i), [KA, 128], BF16) for i in range(2)] for g in range(G)]; t_kpg = TK(2)
            vpg = [sbp("vpg%d" % i, [128, G, 65], BF16) for i in range(2)]; t_vpg = TK(2)
            page = [sbp("page%d" % i, [128, 256]) for i in range(4)]; t_page = TK(4)
            pageb = [sbp("pageb%d" % i, [128, 256], BF16) for i in range(2)]; t_pageb = TK(2)
        wout_sb = sbp("wout_sb", [128, KC, D], BF16); t_wout = TK()
        kcmpT = sbp("kcmpT", [128, SC], BF16); vcmpT = sbp("vcmpT", [128, SC], BF16); t_kcmp = TK(NTC)
        kwinT = [sbp("kwinT%d" % g, [KA, 8, 128], BF16) for g in range(G)]
        vwin = sbp("vwin", [128, 8, G, 65], BF16); t_winst = TK(8)
        kcT = [sbp("kcT%d" % g, [KA, NVT * 128], BF16) for g in range(G)]
        vcaug = sbp("vcaug", [128, NVT, G, 65], BF16); t_cmpst = TK(NVT)
        hst = sbp("hst", [128, HH, HK]); t_hst = TK(HH)
        hsb = sbp("hsb", [128, HH, HK], BF16); t_hsb = TK(HH)
        qn = sbp("qn", [128, 512], BF16); t_qn = TK()
        qT = sbp("qT", [KA, NH, 128], BF16); t_qT = TK()
        tmpA = sbp("tmpA", [128, 512]); t_tmpA = TK()
        kvb = sbp("kvb", [128, 768], BF16); t_kvb = TK()
        gates = sbp("gates", [128, 24]); t_gates = TK()
        Pt = [sbp("Pt%d" % i, [128, 4, 128], BF16) for i in range(2)] ; t_Pt = TK(2)
        nmx = [sbp("nmx%d" % i, [128, 128], BF16) for i in range(3)]; t_nmx = TK(3)
        o_nsa = sbp("o_nsa", [128, NH, HD]); t_onsa = TK()
        sc8 = sbp("sc8", [128, 32]); t_sc8 = TK()
        blk = sbp("blk", [128, NBPAD]); t_blk = TK()
        blk2 = sbp("blk2", [128, NBPAD]); t_blk2 = TK()
        m8 = sbp("m8", [128, 16]); t_m8 = TK()
        nm = sbp("nm", [128, G, NBPAD], BF16); t_nm = TK()
        omix = sbp("omix", [128, D], BF16); t_omix = TK()
        oT = sbp("oT", [128, KC, 128], BF16); t_oT = TK()
        W = [sbp("W%d" % i, [128, 512]) for i in range(5)] + [tmpA]; tW = TK(5) + [t_tmpA]
        rows_sb = W[3]; t_rows = tW[3]
        wrow_sb = W[4]; t_wrow = tW[4]
        hv = sbp("hv", [128, 512], BF16); t_hv = TK()
        hqt = sbp("hqt", [128, 512], BF16); t_hqt = TK()
        hkt = sbp("hkt", [128, 512], BF16); t_hkt = TK()
        hkh = sbp("hkh", [128, 512], BF16); t_hkh = TK()
        hkh3 = sbp("hkh3", [128, 512], BF16); t_hkh3 = TK()
        hqtT3 = sbp("hqtT3", [128, HH, 64], BF16); t_hqtT3 = TK()
        hqtT = sbp("hqtT", [128, HH, 128], BF16); t_hqtT = TK()
        hktT = sbp("hktT", [128, HH, 128], BF16); t_hktT = TK()
        dcs = sbp("dcs", [128, 16]); t_dcs = TK()
        Am = sbp("Am", [128, HH, 128], BF16); t_Am = TK()
        cnt = {"pt": 0, "nx": 0}

        P.barrier()
        rr["nrot"] = 3

        if not sample:
            dma("sp", gnorm[:], nmix[l], [], [t_gn])
            dma("sp", qgs[:], qg[l], [], [t_qg]); dma("sp", kgs[:], kg[l], [], [t_kg])
            ts("dve", qgs[:], qgs[:], 0.125, None, ALU.mult, None, [t_qg], [t_qg])
            dma("sp", ogs[:], ogn[l], [], [t_og])
            if l == 0:
                memset("pool", lb[:], 0.0, t_lb); memset("pool", oml[:], 1.0, t_lb)
            else:
                dma("sp", W[0][:], lbl[:, 0, :], [], [tW[0]]); dma("sp", W[1][:], lbl[:, 1, :], [], [tW[1]])
                tt("dve", lb[:], W[0][:], W[1][:], ALU.subtract, [tW[0], tW[1]], [t_lb])
                act(lb[:], lb[:], AF.Exp, [t_lb], [t_lb])
                ts("dve", lb[:], lb[:], 1.0, None, ALU.add, None, [t_lb], [t_lb])
                recip(lb[:], lb[:], [t_lb], [t_lb])
                ts("dve", oml[:], lb[:], -1.0, 1.0, ALU.mult, ALU.add, [t_lb], [t_lb])
            wv = w_in[l].rearrange("(k p) c -> p k c", p=128)
            for k in range(KC):
                for c0 in range(0, IN_COLS, 1676):
                    dma("pool", win_sb[:, k, c0:c0 + 1676], wv[:, k, c0:c0 + 1676], [], [t_win])
            cwv = cw[l].rearrange("a s d e -> d a s e")
            for half in range(2):
                for a_ in range(2):
                    for s0_ in range(0, 32, 8):
                        dma("pool", cw_sb[64 * half:64 * half + 64, a_, s0_:s0_ + 8, :], cwv[:, a_, s0_:s0_ + 8, :], [], [t_cw])
            dma("sp", pe_raw[0:64, :, :].rearrange("p a s -> p (a s)"), cpe[l].rearrange("a s d -> (a s) d"), [], [t_per])
            ptp, ptpk = psf()
            tr(ptp[0:64, 0:64], pe_raw[0:64, :, :].rearrange("p a s -> p (a s)"), C["identf"][0:64, 0:64], [t_per, CT["identf"]], [ptpk])
            cp("act", peT[0:64, :, :].rearrange("p a s -> p (a s)"), ptp[0:64, 0:64], [ptpk], [t_pe])
            pt, ptk = psf()
            for a in range(2):
                for s in range(32):
                    mm(pt[0:1, a * 64:(a + 1) * 64], peT[0:64, a, s:s + 1], cw_sb[0:64, a, s, :], a == 0 and s == 0, a == 1 and s == 31,
                       [t_pe, t_cw], [ptk])
            cp("act", crow[0:1, :, :].rearrange("p a e -> p (a e)"), pt[0:1, 0:128], [ptk], [t_crow])

        wo = w_out[l].rearrange("(k p) c -> p k c", p=128)
        for k in range(KC):
            dma("pool", wout_sb[:, k, :], wo[:, k, :], [], [t_wout])

        def init_stores():
            memset("pool", kcmpT[:], 0.0, t_kcmp); memset("pool", vcmpT[:], 0.0, t_kcmp)
            for g in range(G):
                memset("pool", kwinT[g][0:64], 0.0, t_winst)
                memset("pool", kcT[g][0:64], 0.0, t_cmpst)
                dma("sp", kcT[g][64:KA, :], cin["kaC"][:, :], [], t_cmpst)
                for sl in range(8):
                    dma("sp", kwinT[g][64:KA, sl, :], cin["kaS"][:, 0:128], [], [t_winst[sl]])
                if not sample:
                    memset("pool", kselT[g][0:64], 0.0, t_ksel[g])
                    dma("sp", kselT[g][64:KA, :], cin["kaS"][:, :], [], t_ksel[g])
                else:
                    for i in range(2):
                        memset("pool", kpg[g][i][0:64], 0.0, [t_kpg[i]])
                        dma("sp", kpg[g][i][64:KA, :], cin["kaS"][:, 0:128], [], [t_kpg[i]])
            memset("pool", vwin[:], 0.0, t_winst); memset("pool", vcaug[:], 0.0, t_cmpst)
            memset("pool", vwin[:, :, :, 64:65], 1.0, t_winst); memset("pool", vcaug[:, :, :, 64:65], 1.0, t_cmpst)
            if not sample:
                memset("pool", vsel[:], 0.0, t_vsel); memset("pool", vsel[:, :, :, 64:65], 1.0, t_vsel)
            else:
                for i in range(2):
                    memset("pool", vpg[i][:], 0.0, [t_vpg[i]]); memset("pool", vpg[i][:, :, 64:65], 1.0, [t_vpg[i]])

        def norm_heads(src_ap, src_toks, nq, nh, hd, gain_ap, gain_tok, out_ap, out_tok, col0):
            w = nh * hd
            act(sq[0:nq, 0:w], src_ap, AF.Square, src_toks, [t_sq])
            P.emit("dve", lambda e: e.tensor_reduce(st1[0:nq, col0:col0 + nh], hv3(sq[0:nq, 0:w], nh), AX.X, ALU.add), [t_sq], [t_st1])
            act(st1[0:nq, col0:col0 + nh], st1[0:nq, col0:col0 + nh], AF.Ln, [t_st1, t_eps], [t_st1], scale=1.0 / hd, bias=epsb[0:nq, 0:1])
            act(st1[0:nq, col0:col0 + nh], st1[0:nq, col0:col0 + nh], AF.Exp, [t_st1], [t_st1], scale=-0.5)
            tt("dve", hv3(tmpA[0:nq, 0:w], nh), hv3(src_ap, nh), st1[0:nq, col0:col0 + nh].unsqueeze(2).to_broadcast([nq, nh, hd]),
               ALU.mult, src_toks + [t_st1], [t_tmpA])
            tt("dve", hv3(out_ap, nh), hv3(tmpA[0:nq, 0:w], nh), gain_ap.unsqueeze(1).to_broadcast([nq, nh, hd]), ALU.mult,
               [t_tmpA, gain_tok], [out_tok])

        def compress_ntile(v, M, key_toks):
            for g in range(G):
                pk, pkt = psf()
                pb0 = 64 * g
                for a, src in ((0, kcmpT), (1, vcmpT)):
                    o = pk[0:M, a * 64:(a + 1) * 64]
                    for s in range(32):
                        st = 2048 * v + s
                        mm(o, src[pb0:pb0 + 64, st:st + 16 * (M - 1) + 1:16], cw_sb[pb0:pb0 + 64, a, s, :], a == 0 and s == 0, False,
                           key_toks + [t_cw], [pkt])
                    mm(o, ones_bf[0:1, 0:M], crow[0:1, a, :], False, a == 1, [t_ones, t_crow], [pkt])
                norm_heads(pk[0:M, 0:64], [pkt], M, 1, 64, kgs[0:M, 0:64], t_kg, qn[0:M, 0:64], t_qn, 12)
                pb, pbt = psb()
                tr(pb[0:64, 0:M], qn[0:M, 0:64], C["identb"][0:M, 0:M], [t_qn, CT["identb"]], [pbt])
                cp("act", kcT[g][0:64, 128 * v:128 * v + M], pb[0:64, 0:M], [pbt], [t_cmpst[v]])
                cp("dve", vcaug[0:M, v, g, 0:64], pk[0:M, 64:128], [pkt], [t_cmpst[v]])

        def score_pair(g, nq, kw, lhsT_k, ktoks, masks, u_blk=None):
            Sb, Sbt = psf()
            S4 = Sb[:].rearrange("p (h i) -> p h i", h=4)
            nmask = len(masks) + (1 if u_blk is not None else 0)
            mm(S4[0:kw, :, 0:nq], lhsT_k, qT[0:KA, 4 * g:4 * g + 4, 0:nq], True, nmask == 0, ktoks + [t_qT], [Sbt])
            k_ = 0
            if u_blk is not None:
                xi = cnt["nx"] % 3; cnt["nx"] += 1
                cp("pool", nmx[xi][0:nq, :].rearrange("p (b j) -> p b j", b=2),
                   nm[0:nq, g, 2 * u_blk:2 * u_blk + 2].unsqueeze(2).to_broadcast([nq, 2, 64]), [t_nm], [t_nmx[xi]])
                k_ += 1
                mm(S4[0:128, :, 0:nq], nmx[xi][0:nq, :], C["identb"][0:nq, 0:nq].unsqueeze(1).to_broadcast([nq, 4, nq]), False, k_ == nmask,
                   [t_nmx[xi], CT["identb"]], [Sbt])
            for (ml, mr, mt) in masks:
                k_ += 1
                mm(S4[0:kw, :, 0:nq], ml, mr.unsqueeze(1).to_broadcast([mr.shape[0], 4, nq]), False, k_ == nmask, mt, [Sbt])
            pi3 = cnt["pt"] % 2; cnt["pt"] += 1
            act(Pt[pi3][0:kw, :, 0:nq], S4[0:kw, :, 0:nq], AF.Exp, [Sbt], [t_Pt[pi3]])
            return pi3

        def pv_pair(nq, kw, pi3, O, Ot, v_rhs, vtoks, first, last):
            for h in range(4):
                mm(O[0:nq, h * 65:(h + 1) * 65], Pt[pi3][0:kw, h, 0:nq], v_rhs, first and h == 0, last and h == 3, [t_Pt[pi3]] + vtoks, [Ot])

        def finish_branch(g, nq, O, Ot, gate_col, first_branch):
            ts("dve", sc8[0:nq, 0:4], O[0:nq, 64:260:65], 1e-30, None, ALU.max, None, [Ot], [t_sc8])
            recip(sc8[0:nq, 4:8], sc8[0:nq, 0:4], [t_sc8], [t_sc8])
            tt("dve", sc8[0:nq, 8:12], sc8[0:nq, 4:8], gates[0:nq, gate_col + 4 * g:gate_col + 4 * g + 4], ALU.mult, [t_sc8, t_gates], [t_sc8])
            for h in range(4):
                if first_branch:
                    ts("dve", o_nsa[0:nq, 4 * g + h, :], O[0:nq, h * 65:h * 65 + 64], sc8[0:nq, 8 + h:9 + h], None, ALU.mult, None,
                       [Ot, t_sc8], [t_onsa])
                else:
                    stt(o_nsa[0:nq, 4 * g + h, :], O[0:nq, h * 65:h * 65 + 64], sc8[0:nq, 8 + h:9 + h], o_nsa[0:nq, 4 * g + h, :],
                        ALU.mult, ALU.add, [Ot, t_sc8, t_onsa], [t_onsa])

        def cmp_topk(g, t, nq, NB, ncmp_valid):
            nmax = min(8 * t + 6, ncmp_valid - 1)
            O, Ot = PSF[3], tPSF[3]
            IMs = [PSF[4], PSF[5]]; IMts = [tPSF[4], tPSF[5]]
            prs = []
            v = 0
            while 128 * v <= nmax:
                prs.append((v, min(128, nmax - 128 * v + 1))); v += 1
            if prs:
                for pi, (v, M) in enumerate(prs):
                    w = t - 16 * v
                    masks = []
                    if w <= 16:
                        masks.append((C["identb"][0:M, 0:M], C["cmpmask"][0:M, w, 0:nq], [CT["identb"], CT["cmpmask"]]))
                    p3 = score_pair(g, nq, M, kcT[g][0:KA, 128 * v:128 * v + M], [t_cmpst[v]], masks)
                    first = pi == 0; last = pi == len(prs) - 1
                    pv_pair(nq, M, p3, O, Ot, vcaug[0:M, v, g, :], [t_cmpst[v]], first, last)
                    for h in range(4):
                        mm(IMs[h // 2][0:nq, (h % 2) * NBPAD:(h % 2 + 1) * NBPAD], Pt[p3][0:M, h, 0:nq], C["Ttab"][0:M, v, :],
                           first and h % 2 == 0, last and h % 2 == 1, [t_Pt[p3], CT["Ttab"]], [IMts[h // 2]])
                finish_branch(g, nq, O, Ot, 0, True)
                for h in range(4):
                    src = IMs[h // 2][0:nq, (h % 2) * NBPAD:(h % 2) * NBPAD + NBPAD]
                    if h == 0:
                        ts("dve", blk[0:nq, :], src, sc8[0:nq, 4:5], None, ALU.mult, None, [IMts[0], t_sc8], [t_blk])
                    else:
                        stt(blk[0:nq, :], src, sc8[0:nq, 4 + h:5 + h], blk[0:nq, :], ALU.mult, ALU.add, [IMts[h // 2], t_sc8, t_blk], [t_blk])
            else:
                memset("dve", o_nsa[0:nq, 4 * g:4 * g + 4, :], 0.0, t_onsa)
                memset("dve", blk[0:nq, :], 0.0, t_blk)
            r0 = 128 - 2 * t
            tt("dve", blk2[0:nq, 0:NB], blk[0:nq, 0:NB], C["Vm"][0:nq, r0:r0 + NB], ALU.mult, [t_blk, CT["Vm"]], [t_blk2])
            tt("dve", blk2[0:nq, 0:NB], blk2[0:nq, 0:NB], C["Cm"][0:nq, r0:r0 + NB], ALU.add, [t_blk2, CT["Cm"]], [t_blk2])
            ts("dve", blk2[0:nq, 0:1], blk2[0:nq, 0:1], 1e4, None, ALU.add, None, [t_blk2], [t_blk2])
            memset("dve", nm[0:nq, g, :], 0.0, t_nm)
            if NB > NSEL:
                cur = blk2
                for rnd in range(NSEL // 8):
                    P.emit("dve", lambda e, cur=cur, rnd=rnd: e.max(m8[0:nq, 8 * rnd:8 * rnd + 8], cur[0:nq, 0:NB]), [t_blk2, t_blk], [t_m8])
                    if rnd < NSEL // 8 - 1:
                        P.emit("dve", lambda e, cur=cur, rnd=rnd: e.match_replace(blk[0:nq, 0:NB], m8[0:nq, 8 * rnd:8 * rnd + 8], cur[0:nq, 0:NB], -1e9),
                               [t_m8, t_blk2, t_blk], [t_blk])
                        cur = blk
                ts("dve", nm[0:nq, g, 0:NB], blk2[0:nq, 0:NB], m8[0:nq, NSEL - 1:NSEL], NEG, ALU.is_lt, ALU.mult, [t_blk2, t_m8], [t_nm])

        def sel_store(g, t, nq):
            O, Ot = PSF[3], tPSF[3]
            for u in range(t + 1):
                masks = []
                if u == t:
                    masks.append((C["identb"][:, :], C["causal"][:, 0:nq], [CT["identb"], CT["causal"]]))
                p3 = score_pair(g, nq, 128, kselT[g][0:KA, 128 * u:128 * u + 128], [t_ksel[g][u]], masks, u_blk=u)
                pv_pair(nq, 128, p3, O, Ot, vsel[:, u, g, :], [t_vsel[u]], u == 0, u == t)
            finish_branch(g, nq, O, Ot, 8, False)

        def win_branch(g, t, nq):
            O, Ot = PSF[3], tPSF[3]
            us = list(range(max(0, t - NWT), t + 1))
            for pi, u in enumerate(us):
                masks = []
                if u == t - NWT:
                    masks.append((C["identb"][:, :], C["lowmask"][:, 0:nq], [CT["identb"], CT["lowmask"]]))
                if u == t:
                    masks.append((C["identb"][:, :], C["causal"][:, 0:nq], [CT["identb"], CT["causal"]]))
                p3 = score_pair(g, nq, 128, kwinT[g][0:KA, u % 8, :], [t_winst[u % 8]], masks)
                pv_pair(nq, 128, p3, O, Ot, vwin[:, u % 8, g, :], [t_winst[u % 8]], pi == 0, pi == len(us) - 1)
            finish_branch(g, nq, O, Ot, 16, False)

        def sel_stream(l_, s, nq):
            Os = [PSF[3], PSF[4]]; Ots = [tPSF[3], tPSF[4]]
            for u in range(NPAGE + 1):
                i2 = u % 2; ip = u % 4; ib = u % 2
                if u < NPAGE:
                    col = s * NPAGE + u
                    gather(page[ip][:], cache[:, :], pidx_v[l_][1][:, col:col + 1], [t_pidx], [t_page[ip]])
                    cp("dve", pageb[ib][:], page[ip][:], [t_page[ip]], [t_pageb[ib]])
                    for g in range(G):
                        pb, pbt = psb()
                        tr(pb[0:64, 0:128], pageb[ib][:, 64 * g:64 * g + 64], C["identb"][:, :], [t_pageb[ib], CT["identb"]], [pbt])
                        cp("act", kpg[g][i2][0:64, :], pb[0:64, 0:128], [pbt], [t_kpg[i2]])
                        memset("pool", kpg[g][i2][64:65, :], float(u), [t_kpg[i2]])
                    cp("pool", vpg[i2][:, :, 0:64], pageb[ib][:, 128:256].rearrange("p (g d) -> p g d", g=G), [t_pageb[ib]], [t_vpg[i2]])
                else:
                    for g in range(G):
                        memset("pool", kpg[g][i2][0:64, :], 0.0, [t_kpg[i2]])
                        pb, pbt = psb()
                        tr(pb[0:64, 0:nq], kvb[0:nq, 256 + 64 * g:256 + 64 * g + 64], C["identb"][0:nq, 0:nq], [t_kvb, CT["identb"]], [pbt])
                        cp("act", kpg[g][i2][0:64, 0:nq], pb[0:64, 0:nq], [pbt], [t_kpg[i2]])
                        memset("pool", kpg[g][i2][64:65, :], float(u), [t_kpg[i2]])
                    memset("pool", vpg[i2][:, :, 0:64], 0.0, [t_vpg[i2]])
                    cp("pool", vpg[i2][0:nq, :, 0:64], kvb[0:nq, 384:512].rearrange("p (g d) -> p g d", g=G), [t_kvb], [t_vpg[i2]])
                for g in range(G):
                    masks = []
                    if u == NPAGE:
                        masks.append((C["identb"][:, :], C["causal"][:, 0:nq], [CT["identb"], CT["causal"]]))
                    p3 = score_pair(g, nq, 128, kpg[g][i2][0:KA, :], [t_kpg[i2]], masks, u_blk=u)
                    pv_pair(nq, 128, p3, Os[g], Ots[g], vpg[i2][:, g, :], [t_vpg[i2]], u == 0, u == NPAGE)
            for g in range(G):
                finish_branch(g, nq, Os[g], Ots[g], 8, False)

        def sigm(zap, ztoks, nq, Wi):
            act(W[Wi][0:nq, :], zap, AF.Exp, ztoks, [tW[Wi]], scale=-1.0)
            ts("dve", W[Wi][0:nq, :], W[Wi][0:nq, :], 1.0, None, ALU.add, None, [tW[Wi]], [tW[Wi]])
            recip(W[Wi][0:nq, :], W[Wi][0:nq, :], [tW[Wi]], [tW[Wi]])

        def hgrn_tile(nq, zc):
            nch = (nq + 31) // 32
            zf, zft = zc(1816, 512)
            sigm(zf, zft, nq, 0)
            tt("dve", W[0][0:nq, :], W[0][0:nq, :], oml[0:nq, :], ALU.mult, [tW[0], t_lb], [tW[0]])
            tt("dve", W[0][0:nq, :], W[0][0:nq, :], lb[0:nq, :], ALU.add, [tW[0], t_lb], [tW[0]])
            ts("dve", W[1][0:nq, :], W[0][0:nq, :], -1.0, 1.0, ALU.mult, ALU.add, [tW[0]], [tW[1]])
            act(W[2][0:nq, :], W[0][0:nq, :], AF.Ln, [tW[0]], [tW[2]])
            zq, zqt = zc(1304, 512)
            sigm(zq, zqt, nq, 0)
            tt("dve", W[3][0:nq, :], zq, W[0][0:nq, :], ALU.mult, zqt + [tW[0]], [tW[3]])
            zg, zgt = zc(2840, 512)
            sigm(zg, zgt, nq, 0)
            tt("dve", W[4][0:nq, :], zg, W[0][0:nq, :], ALU.mult, zgt + [tW[0]], [tW[4]])
            tt("dve", hv3(W[4][0:nq, :], HH), hv3(W[4][0:nq, :], HH), ogs[0:nq, :].unsqueeze(1).to_broadcast([nq, HH, HK]), ALU.mult,
               [tW[4], t_og], [tW[4]])
            zi, zit = zc(2328, 512)
            cp("act", hv[0:nq, :], zi, zit, [t_hv])
            bps, bpt = PSF[3], tPSF[3]
            blp, blt = PSF[4], tPSF[4]
            mm(bps[0:nq, :], C["tri32"][0:nq, 0:nq], W[2][0:nq, :], True, True, [CT["tri32"], tW[2]], [bpt])
            mm(blp[0:nq, :], C["blk32"][0:nq, 0:nq], W[2][0:nq, :], True, True, [CT["blk32"], tW[2]], [blt])
            cp("act", W[5][0:nq, :], bps[0:nq, :], [bpt], [tW[5]])
            act(W[0][0:nq, :], bps[0:nq, :], AF.Exp, [bpt], [tW[0]])
            tt("dve", hqt[0:nq, :], W[3][0:nq, :], W[0][0:nq, :], ALU.mult, [tW[3], tW[0]], [t_hqt])
            act(W[0][0:nq, :], bps[0:nq, :], AF.Exp, [bpt], [tW[0]], scale=-1.0)
            tt("dve", hkt[0:nq, :], W[1][0:nq, :], W[0][0:nq, :], ALU.mult, [tW[1], tW[0]], [t_hkt])
            tt("dve", W[5][0:nq, :], blp[0:nq, :], W[5][0:nq, :], ALU.subtract, [blt, tW[5]], [tW[5]])
            act(W[0][0:nq, :], W[5][0:nq, :], AF.Exp, [tW[5]], [tW[0]])
            tt("dve", hkh[0:nq, :], W[1][0:nq, :], W[0][0:nq, :], ALU.mult, [tW[1], tW[0]], [t_hkh])
            dp, dpt = psf()
            for h in range(HH):
                mm(dp[:, h * 4:h * 4 + nch], W[2][0:nq, h * 128:(h + 1) * 128], C["chunkind"][0:nq, 0:nch], h == 0, h == HH - 1,
                   [tW[2], CT["chunkind"]], [dpt])
            for h in range(HH):
                act(dcs[:, h * 4:h * 4 + nch], dp[:, h * 4:h * 4 + nch], AF.Exp, [dpt], [t_dcs])
            for src, stok, dst, dtok in ((hqt, t_hqt, hqtT, t_hqtT), (hkt, t_hkt, hktT, t_hktT)):
                pb, pbt = psb()
                for h in range(HH):
                    tr(pb[:, h * 128:h * 128 + nq], src[0:nq, h * 128:(h + 1) * 128], C["identb"][0:nq, 0:nq], [stok, CT["identb"]], [pbt])
                cp("act", dst[:, :, 0:nq], pb[:, 0:512].rearrange("p (h i) -> p h i", h=4)[:, :, 0:nq], [pbt], [dtok])
            if nch == 4:
                ts("dve", hkh3[64:128, :], hkh[64:128, :], C["chunkind"][64:128, 3:4], None, ALU.mult, None, [t_hkh, CT["chunkind"]], [t_hkh3])
                cp("pool", hqtT3[:, :, :], hqtT[:, :, 64:128], [t_hqtT], [t_hqtT3])
                memset("pool", hqtT3[:, :, 0:32], 0.0, t_hqtT3)
            ap_, apt = psf()
            A4 = ap_[:].rearrange("p (h i) -> p h i", h=4)
            for h in range(HH):
                mm(A4[0:nq, h, 0:nq], hktT[:, h, 0:nq], hqtT[:, h, 0:nq], h == 0, h == HH - 1, [t_hktT, t_hqtT], [apt])
            tt("dve", Am[0:nq, :, 0:nq], A4[0:nq, :, 0:nq], C["tri32"][0:nq, 0:nq].unsqueeze(1).to_broadcast([nq, HH, nq]), ALU.mult, [apt, CT["tri32"]], [t_Am])
            ops_, opt = PSF[5], tPSF[5]
            for h in range(HH):
                mm(ops_[0:nq, h * 128:(h + 1) * 128], Am[0:nq, h, 0:nq], hv[0:nq, h * 128:(h + 1) * 128], h == 0, False, [t_Am, t_hv], [opt])
                for cidx in range(nch):
                    r0 = 32 * cidx; r1 = min(nq, r0 + 32)
                    cp("act", hsb[:, h, :], hst[:, h, :], [t_hst[h]], [t_hsb[h]])
                    up, upt = psf()
                    if cidx < 3:
                        mm(ops_[r0:r1, h * 128:(h + 1) * 128], hqtT[:, h, r0:r1], hsb[:, h, :], False, h == HH - 1 and cidx == nch - 1,
                           [t_hqtT, t_hsb[h]], [opt])
                        mm(up[:, 0:128], hkh[r0:r1, h * 128:(h + 1) * 128], hv[r0:r1, h * 128:(h + 1) * 128], True, True, [t_hkh, t_hv], [upt])
                    else:
                        mm(ops_[64:128, h * 128:(h + 1) * 128], hqtT3[:, h, :], hsb[:, h, :], False, h == HH - 1 and cidx == nch - 1,
                           [t_hqtT3, t_hsb[h]], [opt])
                        mm(up[:, 0:128], hkh3[64:128, h * 128:(h + 1) * 128], hv[64:128, h * 128:(h + 1) * 128], True, True, [t_hkh3, t_hv], [upt])
                    stt(hst[:, h, :], hst[:, h, :], dcs[:, h * 4 + cidx:h * 4 + cidx + 1], up[:, 0:128], ALU.mult, ALU.add,
                        [t_hst[h], t_dcs, upt], [t_hst[h]])
            act(sq[0:nq, 0:512], ops_[0:nq, :], AF.Square, [opt], [t_sq])
            P.emit("dve", lambda e: e.tensor_reduce(st1[0:nq, 8:12], hv3(sq[0:nq, 0:512], HH), AX.X, ALU.add), [t_sq], [t_st1])
            act(st1[0:nq, 8:12], st1[0:nq, 8:12], AF.Ln, [t_st1, t_eps], [t_st1], scale=1.0 / HK, bias=epsb[0:nq, 0:1])
            act(st1[0:nq, 8:12], st1[0:nq, 8:12], AF.Exp, [t_st1], [t_st1], scale=-0.5)
            tt("dve", hv3(W[0][0:nq, :], HH), hv3(ops_[0:nq, :], HH), st1[0:nq, 8:12].unsqueeze(2).to_broadcast([nq, HH, HK]), ALU.mult,
               [opt, t_st1], [tW[0]])
            tt("dve", omix[0:nq, 512:1024], W[0][0:nq, :], W[4][0:nq, :], ALU.mult, [tW[0], tW[4]], [t_omix])

        def project(nq, x_src):
            dma("sp", xt[0:nq, :], x_src, [], [t_xt])
            act(junk[0:nq, :], xt[0:nq, :], AF.Square, [t_xt], [t_junk, t_st1], accum=st1[0:nq, 15:16])
            act(st1[0:nq, 15:16], st1[0:nq, 15:16], AF.Ln, [t_st1, t_eps], [t_st1], scale=1.0 / D, bias=epsb[0:nq, 0:1])
            act(st1[0:nq, 15:16], st1[0:nq, 15:16], AF.Exp, [t_st1], [t_st1], scale=-0.5)
            stt(hn[0:nq, :], xt[0:nq, :], st1[0:nq, 15:16], gnorm[0:nq, :], ALU.mult, ALU.mult, [t_xt, t_st1, t_gn], [t_hn])
            pb, pbt = psb()
            for k in range(KC):
                tr(pb[:, k * 128:k * 128 + nq], hn[0:nq, k * 128:(k + 1) * 128], C["identb"][0:nq, 0:nq], [t_hn, CT["identb"]], [pbt])
            cp("dve", hT[:, :, 0:nq], pb[:].rearrange("p (k i) -> p k i", k=KC)[:, :, 0:nq], [pbt], [t_hT])

        def zc_mm(nq):
            def zc(c0, wd):
                z, zt = psf()
                for k in range(KC):
                    mm(z[0:nq, 0:wd], hT[:, k, 0:nq], win_sb[:, k, c0:c0 + wd], k == 0, k == KC - 1, [t_hT, t_win], [zt])
                return z[0:nq, 0:wd], [zt]
            return zc

        def mixer_tile(t, nq, zc, kv_dst, win_dst, NB, ncmp_valid, x1_dst, s=None):
            zA, zAt = zc(0, 512)
            norm_heads(zA, zAt, nq, NH, HD, qgs[0:nq, :], t_qg, qn[0:nq, :], t_qn, 0)
            pb, pbt = psb()
            for h in range(NH):
                tr(pb[0:64, h * 128:h * 128 + nq], qn[0:nq, h * 64:(h + 1) * 64], C["identb"][0:nq, 0:nq], [t_qn, CT["identb"]], [pbt])
            cp("act", qT[0:64, :, 0:nq], pb[0:64, :].rearrange("p (h i) -> p h i", h=NH)[:, :, 0:nq], [pbt], [t_qT])
            ts("dve", qT[64:KA, :, 0:nq], C["qabase"][64:KA, :, 0:nq], C["tcol"][64:KA, t:t + 1], None, ALU.mult, None,
               [CT["qabase"], CT["tcol"]], [t_qT])
            zB, zBt = zc(512, 512)
            cp("act", rows_sb[0:nq, 0:256], zB[:, 0:256], zBt, [t_rows])
            cp("act", rows_sb[0:nq, 384:512], zB[:, 384:512], zBt, [t_rows])
            norm_heads(zB[:, 256:384], zBt, nq, G, HD, kgs[0:nq, 64:128], t_kg, rows_sb[0:nq, 256:384], t_rows, 12)
            dma("sp", kv_dst, rows_sb[0:nq, :], [t_rows], [])
            cp("dve", kvb[0:nq, 0:512], rows_sb[0:nq, :], [t_rows], [t_kvb])
            zC, zCt = zc(1024, 280)
            norm_heads(zC[:, 0:128], zCt, nq, G, HD, kgs[0:nq, 128:192], t_kg, wrow_sb[0:nq, 0:128], t_wrow, 12)
            cp("act", wrow_sb[0:nq, 128:256], zC[:, 128:256], zCt, [t_wrow])
            if win_dst is not None:
                dma("sp", win_dst, wrow_sb[0:nq, 0:256], [t_wrow], [])
            cp("dve", kvb[0:nq, 512:768], wrow_sb[0:nq, 0:256], [t_wrow], [t_kvb])
            act(gates[0:nq, :], zC[:, 256:280], AF.Exp, zCt, [t_gates], scale=-1.0)
            ts("dve", gates[0:nq, :], gates[0:nq, :], 1.0, None, ALU.add, None, [t_gates], [t_gates])
            recip(gates[0:nq, :], gates[0:nq, :], [t_gates], [t_gates])
            sl = t % 8
            for g in range(G):
                pb, pbt = psb()
                tr(pb[0:64, 0:nq], kvb[0:nq, 512 + 64 * g:512 + 64 * g + 64], C["identb"][0:nq, 0:nq], [t_kvb, CT["identb"]], [pbt])
                if nq < 128:
                    memset("pool", kwinT[g][0:64, sl, :], 0.0, [t_winst[sl]])
                cp("act", kwinT[g][0:64, sl, 0:nq], pb[0:64, 0:nq], [pbt], [t_winst[sl]])
                memset("pool", kwinT[g][64:65, sl, :], float(t), [t_winst[sl]])
                if not sample:
                    tr(pb[0:64, 128:128 + nq], kvb[0:nq, 256 + 64 * g:256 + 64 * g + 64], C["identb"][0:nq, 0:nq], [t_kvb, CT["identb"]], [pbt])
                    cp("dve", kselT[g][0:64, 128 * t:128 * t + nq], pb[0:64, 128:128 + nq], [pbt], [t_ksel[g][t]])
            if nq < 128:
                memset("pool", vwin[:, sl, :, 0:64], 0.0, [t_winst[sl]])
            cp("pool", vwin[0:nq, sl, :, 0:64], kvb[0:nq, 640:768].rearrange("p (g d) -> p g d", g=G), [t_kvb], [t_winst[sl]])
            if not sample:
                cp("pool", vsel[0:nq, t, :, 0:64], kvb[0:nq, 384:512].rearrange("p (g d) -> p g d", g=G), [t_kvb], [t_vsel[t]])
                pb, pbt = psb()
                tr(pb[:, 0:nq], kvb[0:nq, 0:128], C["identb"][0:nq, 0:nq], [t_kvb, CT["identb"]], [pbt])
                tr(pb[:, 128:128 + nq], kvb[0:nq, 128:256], C["identb"][0:nq, 0:nq], [t_kvb, CT["identb"]], [pbt])
                cp("act", kcmpT[:, 128 * t:128 * t + nq], pb[:, 0:nq], [pbt], [t_kcmp[t]])
                cp("dve", vcmpT[:, 128 * t:128 * t + nq], pb[:, 128:128 + nq], [pbt], [t_kcmp[t]])
                nmax = min(8 * t + 6, ncmp_valid - 1)
                for v in range(NVT):
                    if 128 * v <= nmax and 128 * v + 127 >= 8 * t - 1:
                        M = min(128, nmax - 128 * v + 1)
                        ktoks = [t_kcmp[u] for u in range(16 * v, min(t, 16 * v + 16) + 1)]
                        compress_ntile(v, M, ktoks)
            for g in range(G):
                cmp_topk(g, t, nq, NB, ncmp_valid)
                if not sample:
                    sel_store(g, t, nq)
            if sample:
                sel_stream(l, s, nq)
            for g in range(G):
                win_branch(g, t, nq)
            cp("dve", omix[0:nq, 0:512], o_nsa[0:nq, :, :].rearrange("p h d -> p (h d)"), [t_onsa], [t_omix])
            hgrn_tile(nq, zc)
            pb, pbt = psb()
            for k in range(KC):
                tr(pb[:, k * 128:k * 128 + nq], omix[0:nq, k * 128:(k + 1) * 128], C["identb"][0:nq, 0:nq], [t_omix, CT["identb"]], [pbt])
            cp("act", oT[:, :, 0:nq], pb[:].rearrange("p (k i) -> p k i", k=KC)[:, :, 0:nq], [pbt], [t_oT])
            for half in range(2):
                y, yt = psf()
                for k in range(KC):
                    mm(y[0:nq, :], oT[:, k, 0:nq], wout_sb[:, k, half * 512:(half + 1) * 512], k == 0, k == KC - 1, [t_oT, t_wout], [yt])
                tt("dve", x1t[0:nq, half * 512:(half + 1) * 512], y[0:nq, :], xt[0:nq, half * 512:(half + 1) * 512], ALU.add, [yt, t_xt], [t_x1t])
            dma("sp", x1_dst, x1t[0:nq, :], [t_x1t], [])

        xin_p = xp_in if l == 0 else x2buf[0:S, :]
        xin_s = xs_in if l == 0 else x2buf[S:ROWS, :]
        if not sample:
            init_stores()
            memset("pool", hst[:], 0.0, t_hst)
            for t in range(NT if tiles_limit is None else tiles_limit):
                wd_ = None
                if 128 * t >= S - WIN:
                    w0 = 128 * t - (S - WIN)
                    wd_ = win_p[l, w0:w0 + 128, :]
                project(128, xin_p[128 * t:128 * t + 128, :])
                mixer_tile(t, 128, zc_mm(128), kv_p[l, 128 * t:128 * t + 128, :], wd_, c["NBP"], c["NCP"], x1buf[128 * t:128 * t + 128, :])
            for h in range(HH):
                dma("sp", hg_p[l, h], hst[:, h, :], [t_hst[h]], [])
            project(NSR, xin_s[0:NSR, :])
            zcs = zc_mm(NSR)
            for c0 in range(0, IN_COLS, 512):
                wd = min(512, IN_COLS - c0)
                z, zt = zcs(c0, wd)
                cp("act", tmpA[0:NSR, 0:wd], z, zt, [t_tmpA])
                dma("sp", zst[:, c0:c0 + wd], tmpA[0:NSR, 0:wd], [t_tmpA], [])
        else:
            for s in range(NSEQ):
                init_stores()
                for pg in range(NPAGE):
                    i2 = pg % 2; ip = pg % 4; ib = pg % 2
                    col = s * NPAGE + pg
                    gather(page[ip][:], cache[:, :], pidx_v[l][0][:, col:col + 1], [t_pidx], [t_page[ip]])
                    cp("dve", pageb[ib][:], page[ip][:], [t_page[ip]], [t_pageb[ib]])
                    pb, pbt = psb()
                    tr(pb[:, 0:128], pageb[ib][:, 0:128], C["identb"][:, :], [t_pageb[ib], CT["identb"]], [pbt])
                    tr(pb[:, 128:256], pageb[ib][:, 128:256], C["identb"][:, :], [t_pageb[ib], CT["identb"]], [pbt])
                    cp("act", kcmpT[:, 128 * pg:128 * pg + 128], pb[:, 0:128], [pbt], [t_kcmp[pg]])
                    cp("dve", vcmpT[:, 128 * pg:128 * pg + 128], pb[:, 128:256], [pbt], [t_kcmp[pg]])
                ncs = c["NCS"]
                for v in range((ncs + 127) // 128):
                    M = min(128, ncs - 128 * v)
                    ktoks = [t_kcmp[u] for u in range(16 * v, min(NPAGE - 1, 16 * v + 16) + 1)]
                    compress_ntile(v, M, ktoks)
                for wt in range(NWT):
                    u = TS - NWT + wt
                    i2 = wt % 2; ip = wt % 4; ib = wt % 2
                    dma("sp", page[ip][:], swin[l, s, wt * 128:(wt + 1) * 128, :], [], [t_page[ip]])
                    cp("dve", pageb[ib][:], page[ip][:], [t_page[ip]], [t_pageb[ib]])
                    for g in range(G):
                        pb, pbt = psb()
                        tr(pb[0:64, 0:128], pageb[ib][:, 64 * g:64 * g + 64], C["identb"][:, :], [t_pageb[ib], CT["identb"]], [pbt])
                        cp("act", kwinT[g][0:64, u % 8, :], pb[0:64, 0:128], [pbt], [t_winst[u % 8]])
                        memset("pool", kwinT[g][64:65, u % 8, :], float(u), [t_winst[u % 8]])
                    cp("pool", vwin[:, u % 8, :, 0:64], pageb[ib][:, 128:256].rearrange("p (g d) -> p g d", g=G), [t_pageb[ib]], [t_winst[u % 8]])
                for h in range(HH):
                    dma("sp", hst[:, h, :], shg[l, s, h], [], [t_hst[h]])
                dma("sp", zs[0:DS, :], zst[DS * s:DS * s + DS, :], [], [t_zs])
                dma("sp", xt[0:DS, :], xin_s[DS * s:DS * s + DS, :], [], [t_xt])

                def zc_s(c0, wd):
                    return zs[0:DS, c0:c0 + wd], [t_zs]
                mixer_tile(TS, DS, zc_s, kv_s[l, DS * s:DS * s + DS, :], win_s[l, s, WIN - DS:WIN, :], c["NBS"], c["NCS"],
                           x1buf[S + DS * s:S + DS * s + DS, :], s=s)
                dma("sp", win_s[l, s, 0:WIN - DS, :], swin[l, s, DS:WIN, :], [], [])
                for h in range(HH):
                    dma("sp", hg_s[l, s, h], hst[:, h, :], [t_hst[h]], [])
        P.flush()
        ps_.close()

    fcnt = {"n": 0}

    def ffn_phase(l, row_tiles, last, FG=4):
        fcnt["n"] += 1
        fid = fcnt["n"]
        ps_ = ExitStack()
        moe = (l == 1) and os.environ.get("MOEDBG") != "4"
        TOKB = 512
        NTI = len(row_tiles)
        NCOL = 128 * NTI

        def sbp(name, shape, dt=F32):
            return sbx(ps_, "f%d_%d_" % (l, fid) + name, shape, dt)
        hTf = sbp("hTf", [128, KC, NCOL], BF16); t_hTf = TK()
        yacc = sbp("yacc", [128, NTI, D]); t_yacc = TK(NTI)
        gate_sb = sbp("gate_sb", [128, NTI, 8]); t_gate = TK()
        hT32 = sbp("hT32", [128, KC, 128]); t_hT32 = TK()
        hn32 = sbp("hn32", [128, D]); t_hn32 = TK()
        rt_sb = sbp("rt_sb", [128, KC, 8]); t_rt = TK()
        wg_sb = [sbp("wg_sb%d" % i, [128, KC, FG * 128], BF16) for i in range(2)]
        wu_sb = [sbp("wu_sb%d" % i, [128, KC, FG * 128], BF16) for i in range(2)]
        wd_sb = [sbp("wd_sb%d" % i, [128, FG, D], BF16) for i in range(2)]
        t_wf = TK(2)
        actT = sbp("actT", [128, FG, NCOL], BF16); t_actT = TK()
        sgm2 = [sbp("sgm%d" % i_, [128, 512], BF16) for i_ in range(2)]; t_sgm2 = TK(2)
        scnt = {"n": 0}
        lg = sbp("lg", [128, 32]); t_lg = TK()
        P.barrier()
        rr["nrot"] = 4
        dma("sp", gnorm[:], nffn[l], [], [t_gn])
        DBG5 = os.environ.get("MOEDBG") == "5"
        if moe and not DBG5 and int(os.environ.get("RLVL", "9")) >= 1:
            for k in range(KC):
                dma("sp", rt_sb[:, k, :], rtr[k * 128:(k + 1) * 128, :], [], [t_rt])
        col0 = [128 * i for i in range(NTI)]
        ncols = 128 * (NTI - 1) + row_tiles[-1][1]

        def src_of(r0, n):
            return x1buf[r0:r0 + n, :]

        def dst_of(r0, n):
            if r0 < 0:
                j = -r0 - 1
                return y_p[128 * j:128 * j + 128, :]
            if not last:
                return x2buf[r0:r0 + n, :]
            return y_s[r0 - S:r0 - S + n, :]
        for i, (r0, n) in enumerate(row_tiles):
            if r0 < 0:
                j = -r0 - 1
                gather(xt[0:n, :], x1buf[:, :], ridx[:, j:j + 1], [t_ridx], [t_xt])
            else:
                dma("sp", xt[0:n, :], src_of(r0, n), [], [t_xt])
            act(junk[0:n, :], xt[0:n, :], AF.Square, [t_xt], [t_junk, t_st1], accum=st1[0:n, 15:16])
            act(st1[0:n, 15:16], st1[0:n, 15:16], AF.Ln, [t_st1, t_eps], [t_st1], scale=1.0 / D, bias=epsb[0:n, 0:1])
            act(st1[0:n, 15:16], st1[0:n, 15:16], AF.Exp, [t_st1], [t_st1], scale=-0.5)
            stt(hn32[0:n, :], xt[0:n, :], st1[0:n, 15:16], gnorm[0:n, :], ALU.mult, ALU.mult, [t_xt, t_st1, t_gn], [t_hn32])
            cp("pool", yacc[0:n, i, :], xt[0:n, :], [t_xt], [t_yacc[i]])
            for half in range(2):
                pf, pft = psf()
                for k in range(4):
                    kk = half * 4 + k
                    tr(pf[:, k * 128:k * 128 + n], hn32[0:n, kk * 128:(kk + 1) * 128], C["identf"][0:n, 0:n], [t_hn32, CT["identf"]], [pft])
                v4 = pf[:].rearrange("p (k i) -> p k i", k=4)
                cp("act", hTf[:, half * 4:half * 4 + 4, col0[i]:col0[i] + n], v4[:, :, 0:n], [pft], [t_hTf])
                if moe and not DBG5 and int(os.environ.get("RLVL", "9")) >= 2:
                    cp("act", hT32[:, half * 4:half * 4 + 4, 0:n], v4[:, :, 0:n], [pft], [t_hT32])
            RL = int(os.environ.get("RLVL", "9"))
            if moe and (os.environ.get("MOEDBG") in ("1", "3", "5") or RL < 6):
                memset("dve", gate_sb[0:n, i, :], 0.25, t_gate)
            if moe and os.environ.get("MOEDBG") not in ("1", "3", "5"):
                if RL >= 3:
                    lp, lpt = psf()
                    for k in range(KC):
                        mm(lp[0:n, 0:8], hT32[:, k, 0:n], rt_sb[:, k, :], k == 0, k == KC - 1, [t_hT32, t_rt], [lpt])
                    cp("act", lg[0:n, 0:8], lp[0:n, 0:8], [lpt], [t_lg])
                if RL >= 4:
                    P.emit("dve", lambda e, n=n: e.max(lg[0:n, 8:16], lg[0:n, 0:8]), [t_lg], [t_lg])
                if RL >= 5:
                    tt("dve", lg[0:n, 16:17], lg[0:n, 9:10], lg[0:n, 8:9], ALU.subtract, [t_lg], [t_lg])
                    act(lg[0:n, 16:17], lg[0:n, 16:17], AF.Exp, [t_lg], [t_lg])
                    ts("dve", lg[0:n, 16:17], lg[0:n, 16:17], 1.0, None, ALU.add, None, [t_lg], [t_lg])
                    recip(lg[0:n, 17:18], lg[0:n, 16:17], [t_lg], [t_lg])
                    ts("dve", lg[0:n, 18:19], lg[0:n, 17:18], -1.0, 1.0, ALU.mult, ALU.add, [t_lg], [t_lg])
                if RL >= 6:
                    ts("dve", lg[0:n, 20:28], lg[0:n, 0:8], lg[0:n, 8:9], lg[0:n, 17:18], ALU.is_equal, ALU.mult, [t_lg], [t_lg])
                    ts("dve", gate_sb[0:n, i, :], lg[0:n, 0:8], lg[0:n, 9:10], lg[0:n, 18:19], ALU.is_equal, ALU.mult, [t_lg], [t_gate])
                    tt("dve", gate_sb[0:n, i, :], gate_sb[0:n, i, :], lg[0:n, 20:28], ALU.add, [t_gate, t_lg], [t_gate])
        nfg = (NFC + FG - 1) // FG
        wi = 0
        experts = list(range(NE)) if moe else [0]
        if moe and os.environ.get("MOEDBG") in ("2", "5"):
            experts = [int(x) for x in os.environ.get("EXPL", "0").split(",")]
        for e_ in experts:
            wgd = mwg[e_] if moe else fwg; wud = mwu[e_] if moe else fwu; wdd = mwd[e_] if moe else fwd
            wgv = wgd.rearrange("(k p) f -> p k f", p=128); wuv = wud.rearrange("(k p) f -> p k f", p=128)
            wdv = wdd.rearrange("(c p) d -> p c d", p=128)
            for fg in range(nfg):
                f0 = fg * FG; nf = min(FG, NFC - f0)
                b = wi % 2; wi += 1
                for k in range(KC):
                    dma("pool", wg_sb[b][:, k, 0:nf * 128], wgv[:, k, f0 * 128:(f0 + nf) * 128], [], [t_wf[b]])
                    dma("pool", wu_sb[b][:, k, 0:nf * 128], wuv[:, k, f0 * 128:(f0 + nf) * 128], [], [t_wf[b]])
                for cc in range(nf):
                    dma("pool", wd_sb[b][:, cc, :], wdv[:, f0 + cc, :], [], [t_wf[b]])
                for tb0 in range(0, ncols, TOKB):
                    tw = min(TOKB, ncols - tb0)
                    for cc in range(nf):
                        pa, pat = psf(); pu, put = psf()
                        for k in range(KC):
                            mm(pa[:, 0:tw], wg_sb[b][:, k, cc * 128:(cc + 1) * 128], hTf[:, k, tb0:tb0 + tw], k == 0, k == KC - 1, [t_wf[b], t_hTf], [pat])
                        for k in range(KC):
                            mm(pu[:, 0:tw], wu_sb[b][:, k, cc * 128:(cc + 1) * 128], hTf[:, k, tb0:tb0 + tw], k == 0, k == KC - 1, [t_wf[b], t_hTf], [put])
                        si = scnt["n"] % 2; scnt["n"] += 1
                        act(sgm2[si][:, 0:tw], pa[:, 0:tw], AF.Silu, [pat], [t_sgm2[si]])
                        tt("dve", actT[:, cc, tb0:tb0 + tw], pu[:, 0:tw], sgm2[si][:, 0:tw], ALU.mult, [put, t_sgm2[si]], [t_actT])
                    for i in range(tb0 // 128, (tb0 + tw + 127) // 128):
                        n = row_tiles[i][1]
                        for half in range(2):
                            py, pyt = PSF[4 + half], tPSF[4 + half]
                            for cc in range(nf):
                                mm(py[0:n, :], actT[:, cc, col0[i]:col0[i] + n], wd_sb[b][:, cc, half * 512:(half + 1) * 512], cc == 0, cc == nf - 1,
                                   [t_actT, t_wf[b]], [pyt])
                            ysl = yacc[0:n, i, half * 512:(half + 1) * 512]
                            if moe and not DBG5:
                                stt(ysl, py[0:n, :], gate_sb[0:n, i, e_:e_ + 1], ysl, ALU.mult, ALU.add, [pyt, t_gate, t_yacc[i]], [t_yacc[i]])
                            else:
                                tt("dve", ysl, py[0:n, :], ysl, ALU.add, [pyt, t_yacc[i]], [t_yacc[i]])
        for i, (r0, n) in enumerate(row_tiles):
            dma("sp", dst_of(r0, n), yacc[0:n, i, :], [t_yacc[i]], [])
        P.flush()
        ps_.close()

    dma("sp", pidx[:], ptab[:], [], [t_pidx])
    dma("sp", ridx[:], ridx_in[:], [], [t_ridx])
    P.emit("pool", lambda e: e.iota(iota_p[:], [[0, 1]], base=0, channel_multiplier=1, allow_small_or_imprecise_dtypes=True), (), [t_iota])
    cp("dve", pidxf[:], pidx[:], [t_pidx], [t_pidxf])
    ts("dve", pidxf[:], pidxf[:], 128.0, iota_p[:, 0:1], ALU.mult, ALU.add, [t_pidxf, t_iota], [t_pidxf])
    for l_ in range(2):
        for h_ in range(2):
            ts("dve", W0f[:], pidxf[:], 2.0, float(2 * l_ * NPOOL * 128 + h_), ALU.mult, ALU.add, [t_pidxf], [t_W0f])
            cp("dve", pidx_v[l_][h_][:], W0f[:], [t_W0f], [t_pidx])

    tiles = [(128 * i, 128) for i in range(NT)] + [(S, NSR)]
    nph = 0
    for l in range(2):
        for ph in ("p", "s", "f"):
            if nph >= stop_after:
                break
            nph += 1
            if ph == "p":
                mixer_phase(l, False)
            elif ph == "s":
                mixer_phase(l, True)
            else:
                FG_ = int(os.environ.get("FFNG", "9"))
                tl = tiles if l == 0 else ([(-(j + 1), 128) for j in range(NTH)] + [(S, NSR)])
                if l == 1 and "FFNG" not in os.environ:
                    ffn_phase(l, tl, True, FG=2)
                else:
                    for g0 in range(0, len(tl), FG_):
                        ffn_phase(l, tl[g0:g0 + FG_], l == 1)
    P.barrier()
    P.flush(final=True)
    es.close()
    return nc, consts, P.ninst


def make_in_maps(cfg, inputs, consts, ncores=8):
    c = derive(cfg)
    S, NSEQ, DS, NPAGE, WIN = c["S"], c["NSEQ"], c["DS"], c["NPAGE"], c["WIN"]
    f = lambda a: np.ascontiguousarray(np.asarray(a), dtype=np.float32)
    B = inputs["x_prompt"].shape[0]
    cache = f(inputs["cache_kv"]).reshape(-1, 256)
    rep = lambda a, tail: np.ascontiguousarray(np.broadcast_to(f(a).reshape(2, 1, tail), (2, 128, tail)))
    shared = {
        "cache": cache,
        "nmix": rep(inputs["norm_mix"], D), "nffn": rep(inputs["norm_ffn"], D),
        "w_in": f(inputs["w_in"]), "w_out": f(inputs["w_out"]),
        "qg": rep(inputs["q_gain"], HD), "kg": rep(inputs["k_gain"], 3 * HD),
        "cpe": f(inputs["cmp_pe"]), "cw": f(inputs["cmp_w"]),
        "lbl": np.ascontiguousarray(np.broadcast_to(f(inputs["hgrn_lb_logits"]).reshape(1, 2, HH * HK), (128, 2, HH * HK))),
        "ogn": rep(inputs["hgrn_o_gain"], HK),
        "fwg": f(inputs["ffn_w_gate"])[0], "fwu": f(inputs["ffn_w_up"])[0], "fwd": f(inputs["ffn_w_down"])[0],
        "rtr": f(inputs["moe_router"])[0], "mwg": f(inputs["moe_w_gate"])[0], "mwu": f(inputs["moe_w_up"])[0], "mwd": f(inputs["moe_w_down"])[0],
    }
    for k, v in consts.items():
        shared["c_" + k] = v
    maps = []
    pt = np.asarray(inputs["page_table"]).astype(np.int32)
    for core in range(ncores):
        b = core % B
        s0 = core * NSEQ
        m = dict(shared)
        m["xp"] = f(inputs["x_prompt"][b])
        m["xs"] = f(inputs["x_sample"][s0:s0 + NSEQ]).reshape(NSEQ * DS, D)
        m["swin"] = f(inputs["state_win_kv"][:, s0:s0 + NSEQ]).reshape(2, NSEQ, WIN, 256)
        m["shg"] = f(inputs["state_hgrn"][:, s0:s0 + NSEQ])
        m["ptab"] = np.ascontiguousarray(np.broadcast_to(pt[s0:s0 + NSEQ].reshape(1, NSEQ * NPAGE), (128, NSEQ * NPAGE)))
        hh = core // B
        nth = (S // 128) // 2
        m["ridx"] = ((hh * nth + np.arange(nth)[None, :]) * 128 + np.arange(128)[:, None]).astype(np.int32)
        maps.append(m)
    return maps


def assemble(cfg, r, B, ncores=8):
    c = derive(cfg)
    S, NSEQ, DS, WIN = c["S"], c["NSEQ"], c["DS"], c["WIN"]
    y_p = np.stack([np.concatenate([r[b]["y_p"], r[b + B]["y_p"]], axis=0) for b in range(B)])
    y_s = np.concatenate([r[k]["y_s"].reshape(NSEQ, DS, D) for k in range(ncores)])
    kv_p = np.stack([r[b]["kv_p"] for b in range(B)], axis=1).reshape(2, B, S, 4, G, HD)
    kv_s = np.concatenate([r[k]["kv_s"].reshape(2, NSEQ, DS, 4, G, HD) for k in range(ncores)], axis=1)
    win_p = np.stack([r[b]["win_p"] for b in range(B)], axis=1).reshape(2, B, WIN, 2, G, HD)
    win_s = np.concatenate([r[k]["win_s"].reshape(2, NSEQ, WIN, 2, G, HD) for k in range(ncores)], axis=1)
    hg_p = np.stack([r[b]["hg_p"] for b in range(B)], axis=1)
    hg_s = np.concatenate([r[k]["hg_s"] for k in range(ncores)], axis=1)
    return tuple(np.ascontiguousarray(a, dtype=np.float32) for a in (y_p, y_s, kv_p, kv_s, win_p, win_s, hg_p, hg_s))


_CACHE = {}


def kernel(**inputs):
    cfg = full_cfg()
    if "nc" not in _CACHE:
        _CACHE["nc"] = build_program(cfg)
    nc, consts, _ = _CACHE["nc"]
    maps = make_in_maps(cfg, inputs, consts)
    res = run_bass_kernel_spmd(nc, maps, core_ids=list(range(8)))
    return assemble(cfg, res.results, 4)
```

```python
import os
import numpy as np
import ml_dtypes
from contextlib import ExitStack
import concourse.bass as bass
import concourse.mybir as mybir
from concourse.bass_utils import run_bass_kernel_spmd

F32 = mybir.dt.float32
BF16 = mybir.dt.bfloat16
I32 = mybir.dt.int32
AF = mybir.ActivationFunctionType
ALU = mybir.AluOpType
AX = mybir.AxisListType

D = 1024
KC = 8
NH = 8
G = 2
HD = 64
HH = 4
HK = 128
IN_COLS = 3352
EPS = 1e-6
NEG = -30000.0
SAME_SYNC = True
NDMA = 48
KA = 69


def full_cfg():
    return dict(S=4096, PAST=8192, NSEQ=4, DS=8, DFF=2816, NE=8, NSEL=16, NPOOL=2560, B=4, DB=32, WIN=512)


def derive(cfg):
    c = dict(cfg)
    c["NT"] = c["S"] // 128
    c["TS"] = c["PAST"] // 128
    c["NPAGE"] = c["PAST"] // 128
    c["NBP"] = c["S"] // 64
    c["NBS"] = c["PAST"] // 64 + 1
    c["NCP"] = c["S"] // 16 - 1
    c["NCS"] = c["PAST"] // 16 - 1
    c["NVT"] = (max(c["NCP"], c["NCS"]) + 127) // 128
    c["NBPAD"] = ((max(c["NBP"], c["NBS"]) + 3) // 4) * 4
    c["ND"] = max(c["NT"], c["TS"] + 1)
    c["NFC"] = c["DFF"] // 128
    c["NSR"] = c["NSEQ"] * c["DS"]
    c["ROWS"] = c["S"] + c["NSR"]
    return c


def make_consts(c):
    bf = ml_dtypes.bfloat16
    r = np.arange(128)
    K = {}
    K["identb"] = np.eye(128, dtype=np.float32).astype(bf)
    K["identf"] = np.eye(128, dtype=np.float32)
    same = (r[:, None] // 32) == (r[None, :] // 32)
    K["tri32"] = ((r[:, None] <= r[None, :]) & same).astype(np.float32)
    K["blk32"] = same.astype(np.float32)
    K["chunkind"] = ((r[:, None] // 32) == np.arange(4)[None, :]).astype(np.float32)
    j = r[:, None]; i = r[None, :]
    K["causal"] = np.where(j > i, NEG, 0.0).astype(bf)
    K["lowmask"] = np.where(j <= i, NEG, 0.0).astype(bf)
    cm = np.zeros((128, 17, 128), np.float32)
    for w in range(17):
        cm[:, w, :] = np.where(16 * j + 31 - i > 128 * w, NEG, 0.0)
    K["cmpmask"] = cm.astype(bf)
    slopes = 2.0 ** (-8.0 * (np.arange(NH) + 1.0) / NH)
    qa = np.zeros((128, NH, 128), np.float32)
    for h in range(NH):
        qa[64, h, :] = slopes[h] * 128; qa[65, h, :] = slopes[h]; qa[66, h, :] = slopes[h]
        qa[67, h, :] = -slopes[h] * 128; qa[68, h, :] = -slopes[h] * r
    K["qabase"] = qa.astype(bf)
    tc = np.ones((128, c["ND"] + 1), np.float32)
    tc[67, :] = np.arange(c["ND"] + 1)
    K["tcol"] = tc
    SC = c["S"]
    ka = np.zeros((5, SC), np.float32)
    cols = np.arange(SC)
    ka[0] = cols // 128; ka[1] = cols % 128; ka[2] = 0; ka[3] = 1; ka[4] = 1
    K["kaS"] = ka.astype(bf)
    NV = c["NVT"] * 128
    kc = np.zeros((5, NV), np.float32)
    cols = np.arange(NV)
    kc[0] = 16 * (cols // 128); kc[1] = 16 * (cols % 128); kc[2] = 15.5; kc[3] = 1; kc[4] = 1
    K["kaC"] = kc.astype(bf)
    T = np.zeros((128, c["NVT"], c["NBPAD"]), np.float32)
    taps = [0.5, 1, 1, 1, 0.5]
    for v in range(c["NVT"]):
        for rr in range(128):
            n = 128 * v + rr
            for k in range(5):
                num = n + 1 - k
                if num >= 0 and num % 4 == 0 and num // 4 < c["NBPAD"]:
                    T[rr, v, num // 4] = taps[k]
    K["Ttab"] = T.astype(bf)
    R = 192
    Vm = np.zeros((128, R), np.float32); Cm = np.zeros((128, R), np.float32)
    for ii in range(128):
        hi = 1 if ii >= 64 else 0
        for rr in range(R):
            rel = rr - 128
            valid = rel <= hi
            forced = rel in (hi, hi - 1)
            Vm[ii, rr] = 1.0 if valid else 0.0
            Cm[ii, rr] = (1e4 if forced else 0.0) + (0.0 if valid else -1.0)
    K["Vm"] = Vm; K["Cm"] = Cm
    return K


CDT = {"identb": BF16, "identf": F32, "tri32": F32, "blk32": F32, "chunkind": F32, "trimask4": F32,
       "causal": BF16, "lowmask": BF16, "cmpmask": BF16, "qabase": BF16, "tcol": F32, "kaS": BF16, "kaC": BF16,
       "Ttab": BF16, "Vm": F32, "Cm": F32}


class Tk:
    __slots__ = ("w", "r")

    def __init__(self):
        self.w = {}
        self.r = {}


class Q:
    def __init__(self, name, semi):
        self.name = name; self.semi = semi; self.count = 0; self.waited = {}; self.ops = []


class Prog:
    def __init__(self, nc, es):
        self.nc = nc; self.es = es
        self.sems = []
        self.q = {}
        for n in ("pe", "act", "dve", "pool", "sp"):
            self.q[n] = Q(n, self.new_sem("q" + n))
        self.dma_sems = [self.new_sem("d%d" % i) for i in range(NDMA)]
        self.dma_vals = [0] * NDMA
        self.dma_set = set(self.dma_sems)
        self.dma_rr = 0
        self.ninst = 0

    def new_sem(self, name):
        s = self.es.enter_context(self.nc.semaphore(name))
        self.sems.append(s)
        return len(self.sems) - 1

    def emit(self, eng, fn, r=(), w=(), dma=False):
        q = self.q[eng]
        deps = {}
        dset = self.dma_set

        def add(s, v):
            if deps.get(s, 0) < v:
                deps[s] = v
        for t in r:
            for s, v in t.w.items():
                add(s, v)
        for t in w:
            for s, v in t.w.items():
                if dma and s in dset:
                    continue
                add(s, v)
            for s, v in t.r.items():
                add(s, v)
        if dma:
            slot = self.dma_rr; self.dma_rr = (slot + 1) % NDMA
            sem = self.dma_sems[slot]; pv = self.dma_vals[slot]
            if pv > 0:
                add(sem, pv)
            self.dma_vals[slot] = pv + 16
            ev = (sem, pv + 16); inc = (sem, 16)
        else:
            q.count += 1
            ev = (q.semi, q.count); inc = (q.semi, 1)
        waits = []
        for s, v in deps.items():
            if s == q.semi and not dma and (not SAME_SYNC or eng == "pe"):
                continue
            if q.waited.get(s, 0) >= v:
                continue
            q.waited[s] = v; waits.append((s, v))
        q.ops.append((waits, fn, inc))
        self.ninst += 1 + len(waits)
        s, v = ev
        for t in r:
            if t.r.get(s, 0) < v:
                t.r[s] = v
        for t in w:
            if dma and not t.r:
                if t.w.get(s, 0) < v:
                    t.w[s] = v
            else:
                t.w = {s: v}
            t.r = {}
        return ev

    def barrier(self):
        snap = [(q.semi, q.count) for q in self.q.values() if q.count > 0]
        snap += [(self.dma_sems[i], v) for i, v in enumerate(self.dma_vals) if v > 0]
        for n, q in self.q.items():
            waits = []
            for s, v in snap:
                if s == q.semi:
                    continue
                if q.waited.get(s, 0) >= v:
                    continue
                q.waited[s] = v; waits.append((s, v))
            q.count += 1
            q.ops.append((waits, lambda e: e.nop(), (q.semi, 1)))
            self.ninst += 1 + len(waits)

    def flush(self, final=False):
        P = self
        with self.nc.Block() as block:
            def rp(name):
                def f(e):
                    q = P.q[name]
                    for waits, fn, inc in q.ops:
                        for s, v in waits:
                            e.wait_ge(P.sems[s], v)
                        fn(e).then_inc(P.sems[inc[0]], inc[1])
                    q.ops = []
                    if final and name == "sp":
                        for i, v in enumerate(P.dma_vals):
                            if v > 0:
                                e.wait_ge(P.sems[P.dma_sems[i]], v)
                        for n2, q2 in P.q.items():
                            if n2 != "sp" and q2.count > 0:
                                e.wait_ge(P.sems[q2.semi], q2.count)
                return f
            block.sync(rp("sp")); block.tensor(rp("pe")); block.scalar(rp("act")); block.vector(rp("dve")); block.gpsimd(rp("pool"))


def build_program(cfg, stop_after=99, tiles_limit=None):
    c = derive(cfg)
    S, PAST, NSEQ, DS, DFF, NE, NSEL = c["S"], c["PAST"], c["NSEQ"], c["DS"], c["DFF"], c["NE"], c["NSEL"]
    NT, TS, NPAGE = c["NT"], c["TS"], c["NPAGE"]
    NBPAD, NVT, ND, NFC, ROWS, NSR = c["NBPAD"], c["NVT"], c["ND"], c["NFC"], c["ROWS"], c["NSR"]
    WIN = c["WIN"]; NPOOL = c["NPOOL"]
    NWT = WIN // 128
    consts = make_consts(c)

    nc = bass.Bass("TRN2", target_bir_lowering=False)
    es = ExitStack()
    P = Prog(nc, es)

    def din(name, shape, dt=F32):
        return nc.dram_tensor(name, list(shape), dt, kind="ExternalInput").ap()

    def dout(name, shape, dt=F32):
        return nc.dram_tensor(name, list(shape), dt, kind="ExternalOutput").ap()

    def dint(name, shape, dt=F32):
        return nc.dram_tensor(name, list(shape), dt, kind="Internal").ap()

    xp_in = din("xp", [S, D]); xs_in = din("xs", [NSR, D])
    cache = din("cache", [2 * NPOOL * 128 * 2, 256])
    swin = din("swin", [2, NSEQ, WIN, 256]); shg = din("shg", [2, NSEQ, HH, HK, HK])
    ptab = din("ptab", [128, NSEQ * NPAGE], I32)
    NTH = NT // 2
    ridx_in = din("ridx", [128, NTH], I32)
    nmix = din("nmix", [2, 128, D]); nffn = din("nffn", [2, 128, D])
    w_in = din("w_in", [2, D, IN_COLS]); w_out = din("w_out", [2, D, D])
    qg = din("qg", [2, 128, HD]); kg = din("kg", [2, 128, 3 * HD])
    cpe = din("cpe", [2, 2, 32, HD]); cw = din("cw", [2, 2, 32, HD, HD])
    lbl = din("lbl", [128, 2, HH * HK]); ogn = din("ogn", [2, 128, HK])
    fwg = din("fwg", [D, DFF]); fwu = din("fwu", [D, DFF]); fwd = din("fwd", [DFF, D])
    rtr = din("rtr", [D, NE]); mwg = din("mwg", [NE, D, DFF]); mwu = din("mwu", [NE, D, DFF]); mwd = din("mwd", [NE, DFF, D])
    cin = {k: din("c_" + k, consts[k].shape, CDT[k]) for k in consts}

    y_p = dout("y_p", [S // 2, D]); y_s = dout("y_s", [NSR, D])
    kv_p = dout("kv_p", [2, S, 512]); kv_s = dout("kv_s", [2, NSR, 512])
    win_p = dout("win_p", [2, WIN, 256]); win_s = dout("win_s", [2, NSEQ, WIN, 256])
    hg_p = dout("hg_p", [2, HH, HK, HK]); hg_s = dout("hg_s", [2, NSEQ, HH, HK, HK])
    x1buf = dint("x1buf", [ROWS, D]); x2buf = dint("x2buf", [ROWS, D]); zst = dint("zst", [NSR, IN_COLS])

    def sbx(stack, name, shape, dt=F32):
        return stack.enter_context(nc.sbuf_tensor(name, list(shape), dt))

    def sb(name, shape, dt=F32):
        return sbx(es, name, shape, dt)

    def TK(n=None):
        return Tk() if n is None else [Tk() for _ in range(n)]

    def mm(out, lhsT, rhs, start, stop, r, w):
        P.emit("pe", lambda e: e.matmul(out, lhsT, rhs, start=start, stop=stop, skip_group_check=True), r, w)

    def tr(out, in_, ident, r, w):
        P.emit("pe", lambda e: e.transpose(out, in_, ident), r, w)

    def act(out, in_, func, r, w, bias=None, scale=None, accum=None):
        kw = {}
        if bias is not None: kw["bias"] = bias
        if scale is not None: kw["scale"] = scale
        if accum is not None: kw["accum_out"] = accum
        P.emit("act", lambda e: e.activation(out, in_, func, **kw), r, w)

    def tt(eng, out, in0, in1, op, r, w):
        P.emit(eng, lambda e: e.tensor_tensor(out, in0, in1, op), r, w)

    def ts(eng, out, in0, s1, s2, op0, op1, r, w):
        if op1 is None:
            P.emit(eng, lambda e: e.tensor_scalar(out, in0, s1, None, op0), r, w)
        else:
            P.emit(eng, lambda e: e.tensor_scalar(out, in0, s1, s2, op0, op1), r, w)

    def stt(out, in0, scalar, in1, op0, op1, r, w):
        P.emit("dve", lambda e: e.scalar_tensor_tensor(out, in0, scalar, in1, op0, op1), r, w)

    def cp(eng, out, in_, r, w):
        if eng == "act":
            P.emit("act", lambda e: e.activation(out, in_, AF.Copy), r, w)
        else:
            P.emit(eng, lambda e: e.tensor_copy(out, in_), r, w)

    def recip(out, in_, r, w):
        P.emit("dve", lambda e: e.reciprocal(out, in_), r, w)

    def memset(eng, ap, val, w):
        P.emit(eng, lambda e: e.memset(ap, val), (), w if isinstance(w, list) else [w])

    def dma(eng, out, in_, r, w, slow=False):
        if slow:
            P.emit(eng, lambda e: e.dma_start(out=out, in_=in_, allow_slow_non_contiguous=True), r, w, dma=True)
        else:
            P.emit(eng, lambda e: e.dma_start(out=out, in_=in_), r, w, dma=True)

    def gather(out, in_, idx_ap, r, w):
        P.emit("pool", lambda e: e.indirect_dma_start(out=out, out_offset=None, in_=in_,
                                                       in_offset=bass.IndirectOffsetOnAxis(ap=idx_ap, axis=0)), r, w, dma=True)

    def hv3(ap, h):
        return ap.rearrange("p (h d) -> p h d", h=h)

    C = {}; CT = {}
    for k in consts:
        if k in ("kaS", "kaC"):
            continue
        C[k] = sb("k_" + k, consts[k].shape, CDT[k]); CT[k] = TK()
        dma("sp", C[k][:], cin[k][:], [], [CT[k]])
    ones_bf = sb("ones_bf", [128, 128], BF16); t_ones = TK()
    memset("pool", ones_bf[:], 1.0, t_ones)
    epsb = sb("epsb", [128, 1]); t_eps = TK()
    memset("pool", epsb[:], EPS, t_eps)

    PSF = [es.enter_context(nc.psum_tensor("psf%d" % i, [128, 512], F32)) for i in range(6)]
    tPSF = [TK() for _ in range(6)]
    PSB = [es.enter_context(nc.psum_tensor("psb%d" % i, [128, 1024], BF16)) for i in range(2)]
    tPSB = [TK() for _ in range(2)]
    rr = {"f": 0, "b": 0, "nrot": 3}

    def psf():
        i = rr["f"] % rr["nrot"]; rr["f"] += 1
        return PSF[i], tPSF[i]

    def psb():
        i = rr["b"] % 2; rr["b"] += 1
        return PSB[i], tPSB[i]

    gnorm = sb("gnorm", [128, D]); t_gn = TK()
    qgs = sb("qgs", [128, HD]); kgs = sb("kgs", [128, 3 * HD]); t_qg = TK(); t_kg = TK()
    ogs = sb("ogs", [128, HK]); t_og = TK()
    lb = sb("lb", [128, HH * HK]); oml = sb("oml", [128, HH * HK]); t_lb = TK()
    cw_sb = sb("cw_sb", [128, 2, 32, HD], BF16); t_cw = TK()
    peT = sb("peT", [128, 2, 32], BF16); t_pe = TK()
    pe_raw = sb("pe_raw", [128, 2, 32]); t_per = TK()
    crow = sb("crow", [1, 2, HD], BF16); t_crow = TK()
    xt = sb("xt", [128, D]); t_xt = TK()
    x1t = xt; t_x1t = t_xt
    sq = sb("sq", [128, 512]); t_sq = TK()
    st1 = sb("st1", [128, 16]); t_st1 = TK()
    hn = sb("hn", [128, D], BF16); t_hn = TK()
    junk = hn; t_junk = t_hn
    hT = sb("hT", [128, KC, 128], BF16); t_hT = TK()
    pidx = sb("pidx", [128, NSEQ * NPAGE], I32); t_pidx = TK()
    pidx_v = [[sb("pidx_v%d_%d" % (l_, h_), [128, NSEQ * NPAGE], I32) for h_ in range(2)] for l_ in range(2)]
    pidxf = sb("pidxf", [128, NSEQ * NPAGE]); t_pidxf = TK()
    iota_p = sb("iota_p", [128, 1]); t_iota = TK()
    ridx = sb("ridx_sb", [128, NTH], I32); t_ridx = TK()
    W0f = sb("W0f", [128, NSEQ * NPAGE]); t_W0f = TK()

    def mixer_phase(l, sample):
        ps_ = ExitStack()
        SC = (PAST + 128) if sample else S
        NTC = SC // 128

        def sbp(name, shape, dt=F32):
            return sbx(ps_, ("s%d_" % l if sample else "p%d_" % l) + name, shape, dt)
        if not sample:
            win_sb = sbp("win_sb", [128, KC, IN_COLS], BF16); t_win = TK()
            kselT = [sbp("kselT%d" % g, [KA, SC], BF16) for g in range(G)]; t_ksel = [TK(NTC) for _ in range(G)]
            vsel = sbp("vsel", [128, NTC, G, 65], BF16); t_vsel = TK(NTC)
        else:
            zs = sbp("zs", [8, IN_COLS]); t_zs = TK()
            kpg = [[sbp("kpg%d_%d" % (g, i), [KA, 128], BF16) for i in range(2)] for g in range(G)]; t_kpg = TK(2)
            vpg = [sbp("vpg%d" % i, [128, G, 65], BF16) for i in range(2)]; t_vpg = TK(2)
            page = [sbp("page%d" % i, [128, 256]) for i in range(4)]; t_page = TK(4)
            pageb = [sbp("pageb%d" % i, [128, 256], BF16) for i in range(2)]; t_pageb = TK(2)
        wout_sb = sbp("wout_sb", [128, KC, D], BF16); t_wout = TK()
        kcmpT = sbp("kcmpT", [128, SC], BF16); vcmpT = sbp("vcmpT", [128, SC], BF16); t_kcmp = TK(NTC)
        kwinT = [sbp("kwinT%d" % g, [KA, 8, 128], BF16) for g in range(G)]
        vwin = sbp("vwin", [128, 8, G, 65], BF16); t_winst = TK(8)
        kcT = [sbp("kcT%d" % g, [KA, NVT * 128], BF16) for g in range(G)]
        vcaug = sbp("vcaug", [128, NVT, G, 65], BF16); t_cmpst = TK(NVT)
        hst = sbp("hst", [128, HH, HK]); t_hst = TK(HH)
        hsb = sbp("hsb", [128, HH, HK], BF16); t_hsb = TK(HH)
        qn = sbp("qn", [128, 512], BF16); t_qn = TK()
        qT = sbp("qT", [KA, NH, 128], BF16); t_qT = TK()
        tmpA = sbp("tmpA", [128, 512]); t_tmpA = TK()
        kvb = sbp("kvb", [128, 768], BF16); t_kvb = TK()
        gates = sbp("gates", [128, 24]); t_gates = TK()
        Pt = [sbp("Pt%d" % i, [128, 4, 128], BF16) for i in range(2)] ; t_Pt = TK(2)
        nmx = [sbp("nmx%d" % i, [128, 128], BF16) for i in range(3)]; t_nmx = TK(3)
        o_nsa = sbp("o_nsa", [128, NH, HD]); t_onsa = TK()
        sc8 = sbp("sc8", [128, 32]); t_sc8 = TK()
        blk = sbp("blk", [128, NBPAD]); t_blk = TK()
        blk2 = sbp("blk2", [128, NBPAD]); t_blk2 = TK()
        m8 = sbp("m8", [128, 16]); t_m8 = TK()
        nm = sbp("nm", [128, G, NBPAD], BF16); t_nm = TK()
        omix = sbp("omix", [128, D], BF16); t_omix = TK()
        oT = sbp("oT", [128, KC, 128], BF16); t_oT = TK()
        W = [sbp("W%d" % i, [128, 512]) for i in range(5)] + [tmpA]; tW = TK(5) + [t_tmpA]
        rows_sb = W[3]; t_rows = tW[3]
        wrow_sb = W[4]; t_wrow = tW[4]
        hv = sbp("hv", [128, 512], BF16); t_hv = TK()
        hqt = sbp("hqt", [128, 512], BF16); t_hqt = TK()
        hkt = sbp("hkt", [128, 512], BF16); t_hkt = TK()
        hkh = sbp("hkh", [128, 512], BF16); t_hkh = TK()
        hkh3 = sbp("hkh3", [128, 512], BF16); t_hkh3 = TK()
        hqtT3 = sbp("hqtT3", [128, HH, 64], BF16); t_hqtT3 = TK()
        hqtT = sbp("hqtT", [128, HH, 128], BF16); t_hqtT = TK()
        hktT = sbp("hktT", [128, HH, 128], BF16); t_hktT = TK()
        dcs = sbp("dcs", [128, 16]); t_dcs = TK()
        Am = sbp("Am", [128, HH, 128], BF16); t_Am = TK()
        cnt = {"pt": 0, "nx": 0}

        P.barrier()
        rr["nrot"] = 3

        if not sample:
            dma("sp", gnorm[:], nmix[l], [], [t_gn])
            dma("sp", qgs[:], qg[l], [], [t_qg]); dma("sp", kgs[:], kg[l], [], [t_kg])
            ts("dve", qgs[:], qgs[:], 0.125, None, ALU.mult, None, [t_qg], [t_qg])
            dma("sp", ogs[:], ogn[l], [], [t_og])
            if l == 0:
                memset("pool", lb[:], 0.0, t_lb); memset("pool", oml[:], 1.0, t_lb)
            else:
                dma("sp", W[0][:], lbl[:, 0, :], [], [tW[0]]); dma("sp", W[1][:], lbl[:, 1, :], [], [tW[1]])
                tt("dve", lb[:], W[0][:], W[1][:], ALU.subtract, [tW[0], tW[1]], [t_lb])
                act(lb[:], lb[:], AF.Exp, [t_lb], [t_lb])
                ts("dve", lb[:], lb[:], 1.0, None, ALU.add, None, [t_lb], [t_lb])
                recip(lb[:], lb[:], [t_lb], [t_lb])
                ts("dve", oml[:], lb[:], -1.0, 1.0, ALU.mult, ALU.add, [t_lb], [t_lb])
            wv = w_in[l].rearrange("(k p) c -> p k c", p=128)
            for k in range(KC):
                for c0 in range(0, IN_COLS, 1676):
                    dma("pool", win_sb[:, k, c0:c0 + 1676], wv[:, k, c0:c0 + 1676], [], [t_win])
            cwv = cw[l].rearrange("a s d e -> d a s e")
            for half in range(2):
                for a_ in range(2):
                    for s0_ in range(0, 32, 8):
                        dma("pool", cw_sb[64 * half:64 * half + 64, a_, s0_:s0_ + 8, :], cwv[:, a_, s0_:s0_ + 8, :], [], [t_cw])
            dma("sp", pe_raw[0:64, :, :].rearrange("p a s -> p (a s)"), cpe[l].rearrange("a s d -> (a s) d"), [], [t_per])
            ptp, ptpk = psf()
            tr(ptp[0:64, 0:64], pe_raw[0:64, :, :].rearrange("p a s -> p (a s)"), C["identf"][0:64, 0:64], [t_per, CT["identf"]], [ptpk])
            cp("act", peT[0:64, :, :].rearrange("p a s -> p (a s)"), ptp[0:64, 0:64], [ptpk], [t_pe])
            pt, ptk = psf()
            for a in range(2):
                for s in range(32):
                    mm(pt[0:1, a * 64:(a + 1) * 64], peT[0:64, a, s:s + 1], cw_sb[0:64, a, s, :], a == 0 and s == 0, a == 1 and s == 31,
                       [t_pe, t_cw], [ptk])
            cp("act", crow[0:1, :, :].rearrange("p a e -> p (a e)"), pt[0:1, 0:128], [ptk], [t_crow])

        wo = w_out[l].rearrange("(k p) c -> p k c", p=128)
        for k in range(KC):
            dma("pool", wout_sb[:, k, :], wo[:, k, :], [], [t_wout])

        def init_stores():
            memset("pool", kcmpT[:], 0.0, t_kcmp); memset("pool", vcmpT[:], 0.0, t_kcmp)
            for g in range(G):
                memset("pool", kwinT[g][0:64], 0.0, t_winst)
                memset("pool", kcT[g][0:64], 0.0, t_cmpst)
                dma("sp", kcT[g][64:KA, :], cin["kaC"][:, :], [], t_cmpst)
                for sl in range(8):
                    dma("sp", kwinT[g][64:KA, sl, :], cin["kaS"][:, 0:128], [], [t_winst[sl]])
                if not sample:
                    memset("pool", kselT[g][0:64], 0.0, t_ksel[g])
                    dma("sp", kselT[g][64:KA, :], cin["kaS"][:, :], [], t_ksel[g])
                else:
                    for i in range(2):
                        memset("pool", kpg[g][i][0:64], 0.0, [t_kpg[i]])
                        dma("sp", kpg[g][i][64:KA, :], cin["kaS"][:, 0:128], [], [t_kpg[i]])
            memset("pool", vwin[:], 0.0, t_winst); memset("pool", vcaug[:], 0.0, t_cmpst)
            memset("pool", vwin[:, :, :, 64:65], 1.0, t_winst); memset("pool", vcaug[:, :, :, 64:65], 1.0, t_cmpst)
            if not sample:
                memset("pool", vsel[:], 0.0, t_vsel); memset("pool", vsel[:, :, :, 64:65], 1.0, t_vsel)
            else:
                for i in range(2):
                    memset("pool", vpg[i][:], 0.0, [t_vpg[i]]); memset("pool", vpg[i][:, :, 64:65], 1.0, [t_vpg[i]])

        def norm_heads(src_ap, src_toks, nq, nh, hd, gain_ap, gain_tok, out_ap, out_tok, col0):
            w = nh * hd
            act(sq[0:nq, 0:w], src_ap, AF.Square, src_toks, [t_sq])
            P.emit("dve", lambda e: e.tensor_reduce(st1[0:nq, col0:col0 + nh], hv3(sq[0:nq, 0:w], nh), AX.X, ALU.add), [t_sq], [t_st1])
            act(st1[0:nq, col0:col0 + nh], st1[0:nq, col0:col0 + nh], AF.Ln, [t_st1, t_eps], [t_st1], scale=1.0 / hd, bias=epsb[0:nq, 0:1])
            act(st1[0:nq, col0:col0 + nh], st1[0:nq, col0:col0 + nh], AF.Exp, [t_st1], [t_st1], scale=-0.5)
            tt("dve", hv3(tmpA[0:nq, 0:w], nh), hv3(src_ap, nh), st1[0:nq, col0:col0 + nh].unsqueeze(2).to_broadcast([nq, nh, hd]),
               ALU.mult, src_toks + [t_st1], [t_tmpA])
            tt("dve", hv3(out_ap, nh), hv3(tmpA[0:nq, 0:w], nh), gain_ap.unsqueeze(1).to_broadcast([nq, nh, hd]), ALU.mult,
               [t_tmpA, gain_tok], [out_tok])

        def compress_ntile(v, M, key_toks):
            for g in range(G):
                pk, pkt = psf()
                pb0 = 64 * g
                for a, src in ((0, kcmpT), (1, vcmpT)):
                    o = pk[0:M, a * 64:(a + 1) * 64]
                    for s in range(32):
                        st = 2048 * v + s
                        mm(o, src[pb0:pb0 + 64, st:st + 16 * (M - 1) + 1:16], cw_sb[pb0:pb0 + 64, a, s, :], a == 0 and s == 0, False,
                           key_toks + [t_cw], [pkt])
                    mm(o, ones_bf[0:1, 0:M], crow[0:1, a, :], False, a == 1, [t_ones, t_crow], [pkt])
                norm_heads(pk[0:M, 0:64], [pkt], M, 1, 64, kgs[0:M, 0:64], t_kg, qn[0:M, 0:64], t_qn, 12)
                pb, pbt = psb()
                tr(pb[0:64, 0:M], qn[0:M, 0:64], C["identb"][0:M, 0:M], [t_qn, CT["identb"]], [pbt])
                cp("act", kcT[g][0:64, 128 * v:128 * v + M], pb[0:64, 0:M], [pbt], [t_cmpst[v]])
                cp("dve", vcaug[0:M, v, g, 0:64], pk[0:M, 64:128], [pkt], [t_cmpst[v]])

        def score_pair(g, nq, kw, lhsT_k, ktoks, masks, u_blk=None):
            Sb, Sbt = psf()
            S4 = Sb[:].rearrange("p (h i) -> p h i", h=4)
            nmask = len(masks) + (1 if u_blk is not None else 0)
            mm(S4[0:kw, :, 0:nq], lhsT_k, qT[0:KA, 4 * g:4 * g + 4, 0:nq], True, nmask == 0, ktoks + [t_qT], [Sbt])
            k_ = 0
            if u_blk is not None:
                xi = cnt["nx"] % 3; cnt["nx"] += 1
                cp("pool", nmx[xi][0:nq, :].rearrange("p (b j) -> p b j", b=2),
                   nm[0:nq, g, 2 * u_blk:2 * u_blk + 2].unsqueeze(2).to_broadcast([nq, 2, 64]), [t_nm], [t_nmx[xi]])
                k_ += 1
                mm(S4[0:128, :, 0:nq], nmx[xi][0:nq, :], C["identb"][0:nq, 0:nq].unsqueeze(1).to_broadcast([nq, 4, nq]), False, k_ == nmask,
                   [t_nmx[xi], CT["identb"]], [Sbt])
            for (ml, mr, mt) in masks:
                k_ += 1
                mm(S4[0:kw, :, 0:nq], ml, mr.unsqueeze(1).to_broadcast([mr.shape[0], 4, nq]), False, k_ == nmask, mt, [Sbt])
            pi3 = cnt["pt"] % 2; cnt["pt"] += 1
            act(Pt[pi3][0:kw, :, 0:nq], S4[0:kw, :, 0:nq], AF.Exp, [Sbt], [t_Pt[pi3]])
            return pi3

        def pv_pair(nq, kw, pi3, O, Ot, v_rhs, vtoks, first, last):
            for h in range(4):
                mm(O[0:nq, h * 65:(h + 1) * 65], Pt[pi3][0:kw, h, 0:nq], v_rhs, first and h == 0, last and h == 3, [t_Pt[pi3]] + vtoks, [Ot])

        def finish_branch(g, nq, O, Ot, gate_col, first_branch):
            ts("dve", sc8[0:nq, 0:4], O[0:nq, 64:260:65], 1e-30, None, ALU.max, None, [Ot], [t_sc8])
            recip(sc8[0:nq, 4:8], sc8[0:nq, 0:4], [t_sc8], [t_sc8])
            tt("dve", sc8[0:nq, 8:12], sc8[0:nq, 4:8], gates[0:nq, gate_col + 4 * g:gate_col + 4 * g + 4], ALU.mult, [t_sc8, t_gates], [t_sc8])
            for h in range(4):
                if first_branch:
                    ts("dve", o_nsa[0:nq, 4 * g + h, :], O[0:nq, h * 65:h * 65 + 64], sc8[0:nq, 8 + h:9 + h], None, ALU.mult, None,
                       [Ot, t_sc8], [t_onsa])
                else:
                    stt(o_nsa[0:nq, 4 * g + h, :], O[0:nq, h * 65:h * 65 + 64], sc8[0:nq, 8 + h:9 + h], o_nsa[0:nq, 4 * g + h, :],
                        ALU.mult, ALU.add, [Ot, t_sc8, t_onsa], [t_onsa])

        def cmp_topk(g, t, nq, NB, ncmp_valid):
            nmax = min(8 * t + 6, ncmp_valid - 1)
            O, Ot = PSF[3], tPSF[3]
            IMs = [PSF[4], PSF[5]]; IMts = [tPSF[4], tPSF[5]]
            prs = []
            v = 0
            while 128 * v <= nmax:
                prs.append((v, min(128, nmax - 128 * v + 1))); v += 1
            if prs:
                for pi, (v, M) in enumerate(prs):
                    w = t - 16 * v
                    masks = []
                    if w <= 16:
                        masks.append((C["identb"][0:M, 0:M], C["cmpmask"][0:M, w, 0:nq], [CT["identb"], CT["cmpmask"]]))
                    p3 = score_pair(g, nq, M, kcT[g][0:KA, 128 * v:128 * v + M], [t_cmpst[v]], masks)
                    first = pi == 0; last = pi == len(prs) - 1
                    pv_pair(nq, M, p3, O, Ot, vcaug[0:M, v, g, :], [t_cmpst[v]], first, last)
                    for h in range(4):
                        mm(IMs[h // 2][0:nq, (h % 2) * NBPAD:(h % 2 + 1) * NBPAD], Pt[p3][0:M, h, 0:nq], C["Ttab"][0:M, v, :],
                           first and h % 2 == 0, last and h % 2 == 1, [t_Pt[p3], CT["Ttab"]], [IMts[h // 2]])
                finish_branch(g, nq, O, Ot, 0, True)
                for h in range(4):
                    src = IMs[h // 2][0:nq, (h % 2) * NBPAD:(h % 2) * NBPAD + NBPAD]
                    if h == 0:
                        ts("dve", blk[0:nq, :], src, sc8[0:nq, 4:5], None, ALU.mult, None, [IMts[0], t_sc8], [t_blk])
                    else:
                        stt(blk[0:nq, :], src, sc8[0:nq, 4 + h:5 + h], blk[0:nq, :], ALU.mult, ALU.add, [IMts[h // 2], t_sc8, t_blk], [t_blk])
            else:
                memset("dve", o_nsa[0:nq, 4 * g:4 * g + 4, :], 0.0, t_onsa)
                memset("dve", blk[0:nq, :], 0.0, t_blk)
            r0 = 128 - 2 * t
            tt("dve", blk2[0:nq, 0:NB], blk[0:nq, 0:NB], C["Vm"][0:nq, r0:r0 + NB], ALU.mult, [t_blk, CT["Vm"]], [t_blk2])
            tt("dve", blk2[0:nq, 0:NB], blk2[0:nq, 0:NB], C["Cm"][0:nq, r0:r0 + NB], ALU.add, [t_blk2, CT["Cm"]], [t_blk2])
            ts("dve", blk2[0:nq, 0:1], blk2[0:nq, 0:1], 1e4, None, ALU.add, None, [t_blk2], [t_blk2])
            memset("dve", nm[0:nq, g, :], 0.0, t_nm)
            if NB > NSEL:
                cur = blk2
                for rnd in range(NSEL // 8):
                    P.emit("dve", lambda e, cur=cur, rnd=rnd: e.max(m8[0:nq, 8 * rnd:8 * rnd + 8], cur[0:nq, 0:NB]), [t_blk2, t_blk], [t_m8])
                    if rnd < NSEL // 8 - 1:
                        P.emit("dve", lambda e, cur=cur, rnd=rnd: e.match_replace(blk[0:nq, 0:NB], m8[0:nq, 8 * rnd:8 * rnd + 8], cur[0:nq, 0:NB], -1e9),
                               [t_m8, t_blk2, t_blk], [t_blk])
                        cur = blk
                ts("dve", nm[0:nq, g, 0:NB], blk2[0:nq, 0:NB], m8[0:nq, NSEL - 1:NSEL], NEG, ALU.is_lt, ALU.mult, [t_blk2, t_m8], [t_nm])

        def sel_store(g, t, nq):
            O, Ot = PSF[3], tPSF[3]
            for u in range(t + 1):
                masks = []
                if u == t:
                    masks.append((C["identb"][:, :], C["causal"][:, 0:nq], [CT["identb"], CT["causal"]]))
                p3 = score_pair(g, nq, 128, kselT[g][0:KA, 128 * u:128 * u + 128], [t_ksel[g][u]], masks, u_blk=u)
                pv_pair(nq, 128, p3, O, Ot, vsel[:, u, g, :], [t_vsel[u]], u == 0, u == t)
            finish_branch(g, nq, O, Ot, 8, False)

        def win_branch(g, t, nq):
            O, Ot = PSF[3], tPSF[3]
            us = list(range(max(0, t - NWT), t + 1))
            for pi, u in enumerate(us):
                masks = []
                if u == t - NWT:
                    masks.append((C["identb"][:, :], C["lowmask"][:, 0:nq], [CT["identb"], CT["lowmask"]]))
                if u == t:
                    masks.append((C["identb"][:, :], C["causal"][:, 0:nq], [CT["identb"], CT["causal"]]))
                p3 = score_pair(g, nq, 128, kwinT[g][0:KA, u % 8, :], [t_winst[u % 8]], masks)
                pv_pair(nq, 128, p3, O, Ot, vwin[:, u % 8, g, :], [t_winst[u % 8]], pi == 0, pi == len(us) - 1)
            finish_branch(g, nq, O, Ot, 16, False)

        def sel_stream(l_, s, nq):
            Os = [PSF[3], PSF[4]]; Ots = [tPSF[3], tPSF[4]]
            for u in range(NPAGE + 1):
                i2 = u % 2; ip = u % 4; ib = u % 2
                if u < NPAGE:
                    col = s * NPAGE + u
                    gather(page[ip][:], cache[:, :], pidx_v[l_][1][:, col:col + 1], [t_pidx], [t_page[ip]])
                    cp("dve", pageb[ib][:], page[ip][:], [t_page[ip]], [t_pageb[ib]])
                    for g in range(G):
                        pb, pbt = psb()
                        tr(pb[0:64, 0:128], pageb[ib][:, 64 * g:64 * g + 64], C["identb"][:, :], [t_pageb[ib], CT["identb"]], [pbt])
                        cp("act", kpg[g][i2][0:64, :], pb[0:64, 0:128], [pbt], [t_kpg[i2]])
                        memset("pool", kpg[g][i2][64:65, :], float(u), [t_kpg[i2]])
                    cp("pool", vpg[i2][:, :, 0:64], pageb[ib][:, 128:256].rearrange("p (g d) -> p g d", g=G), [t_pageb[ib]], [t_vpg[i2]])
                else:
                    for g in range(G):
                        memset("pool", kpg[g][i2][0:64, :], 0.0, [t_kpg[i2]])
                        pb, pbt = psb()
                        tr(pb[0:64, 0:nq], kvb[0:nq, 256 + 64 * g:256 + 64 * g + 64], C["identb"][0:nq, 0:nq], [t_kvb, CT["identb"]], [pbt])
                        cp("act", kpg[g][i2][0:64, 0:nq], pb[0:64, 0:nq], [pbt], [t_kpg[i2]])
                        memset("pool", kpg[g][i2][64:65, :], float(u), [t_kpg[i2]])
                    memset("pool", vpg[i2][:, :, 0:64], 0.0, [t_vpg[i2]])
                    cp("pool", vpg[i2][0:nq, :, 0:64], kvb[0:nq, 384:512].rearrange("p (g d) -> p g d", g=G), [t_kvb], [t_vpg[i2]])
                for g in range(G):
                    masks = []
                    if u == NPAGE:
                        masks.append((C["identb"][:, :], C["causal"][:, 0:nq], [CT["identb"], CT["causal"]]))
                    p3 = score_pair(g, nq, 128, kpg[g][i2][0:KA, :], [t_kpg[i2]], masks, u_blk=u)
                    pv_pair(nq, 128, p3, Os[g], Ots[g], vpg[i2][:, g, :], [t_vpg[i2]], u == 0, u == NPAGE)
            for g in range(G):
                finish_branch(g, nq, Os[g], Ots[g], 8, False)

        def sigm(zap, ztoks, nq, Wi):
            act(W[Wi][0:nq, :], zap, AF.Exp, ztoks, [tW[Wi]], scale=-1.0)
            ts("dve", W[Wi][0:nq, :], W[Wi][0:nq, :], 1.0, None, ALU.add, None, [tW[Wi]], [tW[Wi]])
            recip(W[Wi][0:nq, :], W[Wi][0:nq, :], [tW[Wi]], [tW[Wi]])

        def hgrn_tile(nq, zc):
            nch = (nq + 31) // 32
            zf, zft = zc(1816, 512)
            sigm(zf, zft, nq, 0)
            tt("dve", W[0][0:nq, :], W[0][0:nq, :], oml[0:nq, :], ALU.mult, [tW[0], t_lb], [tW[0]])
            tt("dve", W[0][0:nq, :], W[0][0:nq, :], lb[0:nq, :], ALU.add, [tW[0], t_lb], [tW[0]])
            ts("dve", W[1][0:nq, :], W[0][0:nq, :], -1.0, 1.0, ALU.mult, ALU.add, [tW[0]], [tW[1]])
            act(W[2][0:nq, :], W[0][0:nq, :], AF.Ln, [tW[0]], [tW[2]])
            zq, zqt = zc(1304, 512)
            sigm(zq, zqt, nq, 0)
            tt("dve", W[3][0:nq, :], zq, W[0][0:nq, :], ALU.mult, zqt + [tW[0]], [tW[3]])
            zg, zgt = zc(2840, 512)
            sigm(zg, zgt, nq, 0)
            tt("dve", W[4][0:nq, :], zg, W[0][0:nq, :], ALU.mult, zgt + [tW[0]], [tW[4]])
            tt("dve", hv3(W[4][0:nq, :], HH), hv3(W[4][0:nq, :], HH), ogs[0:nq, :].unsqueeze(1).to_broadcast([nq, HH, HK]), ALU.mult,
               [tW[4], t_og], [tW[4]])
            zi, zit = zc(2328, 512)
            cp("act", hv[0:nq, :], zi, zit, [t_hv])
            bps, bpt = PSF[3], tPSF[3]
            blp, blt = PSF[4], tPSF[4]
            mm(bps[0:nq, :], C["tri32"][0:nq, 0:nq], W[2][0:nq, :], True, True, [CT["tri32"], tW[2]], [bpt])
            mm(blp[0:nq, :], C["blk32"][0:nq, 0:nq], W[2][0:nq, :], True, True, [CT["blk32"], tW[2]], [blt])
            cp("act", W[5][0:nq, :], bps[0:nq, :], [bpt], [tW[5]])
            act(W[0][0:nq, :], bps[0:nq, :], AF.Exp, [bpt], [tW[0]])
            tt("dve", hqt[0:nq, :], W[3][0:nq, :], W[0][0:nq, :], ALU.mult, [tW[3], tW[0]], [t_hqt])
            act(W[0][0:nq, :], bps[0:nq, :], AF.Exp, [bpt], [tW[0]], scale=-1.0)
            tt("dve", hkt[0:nq, :], W[1][0:nq, :], W[0][0:nq, :], ALU.mult, [tW[1], tW[0]], [t_hkt])
            tt("dve", W[5][0:nq, :], blp[0:nq, :], W[5][0:nq, :], ALU.subtract, [blt, tW[5]], [tW[5]])
            act(W[0][0:nq, :], W[5][0:nq, :], AF.Exp, [tW[5]], [tW[0]])
            tt("dve", hkh[0:nq, :], W[1][0:nq, :], W[0][0:nq, :], ALU.mult, [tW[1], tW[0]], [t_hkh])
            dp, dpt = psf()
            for h in range(HH):
                mm(dp[:, h * 4:h * 4 + nch], W[2][0:nq, h * 128:(h + 1) * 128], C["chunkind"][0:nq, 0:nch], h == 0, h == HH - 1,
                   [tW[2], CT["chunkind"]], [dpt])
            for h in range(HH):
                act(dcs[:, h * 4:h * 4 + nch], dp[:, h * 4:h * 4 + nch], AF.Exp, [dpt], [t_dcs])
            for src, stok, dst, dtok in ((hqt, t_hqt, hqtT, t_hqtT), (hkt, t_hkt, hktT, t_hktT)):
                pb, pbt = psb()
                for h in range(HH):
                    tr(pb[:, h * 128:h * 128 + nq], src[0:nq, h * 128:(h + 1) * 128], C["identb"][0:nq, 0:nq], [stok, CT["identb"]], [pbt])
                cp("act", dst[:, :, 0:nq], pb[:, 0:512].rearrange("p (h i) -> p h i", h=4)[:, :, 0:nq], [pbt], [dtok])
            if nch == 4:
                ts("dve", hkh3[64:128, :], hkh[64:128, :], C["chunkind"][64:128, 3:4], None, ALU.mult, None, [t_hkh, CT["chunkind"]], [t_hkh3])
                cp("pool", hqtT3[:, :, :], hqtT[:, :, 64:128], [t_hqtT], [t_hqtT3])
                memset("pool", hqtT3[:, :, 0:32], 0.0, t_hqtT3)
            ap_, apt = psf()
            A4 = ap_[:].rearrange("p (h i) -> p h i", h=4)
            for h in range(HH):
                mm(A4[0:nq, h, 0:nq], hktT[:, h, 0:nq], hqtT[:, h, 0:nq], h == 0, h == HH - 1, [t_hktT, t_hqtT], [apt])
            tt("dve", Am[0:nq, :, 0:nq], A4[0:nq, :, 0:nq], C["tri32"][0:nq, 0:nq].unsqueeze(1).to_broadcast([nq, HH, nq]), ALU.mult, [apt, CT["tri32"]], [t_Am])
            ops_, opt = PSF[5], tPSF[5]
            for h in range(HH):
                mm(ops_[0:nq, h * 128:(h + 1) * 128], Am[0:nq, h, 0:nq], hv[0:nq, h * 128:(h + 1) * 128], h == 0, False, [t_Am, t_hv], [opt])
                for cidx in range(nch):
                    r0 = 32 * cidx; r1 = min(nq, r0 + 32)
                    cp("act", hsb[:, h, :], hst[:, h, :], [t_hst[h]], [t_hsb[h]])
                    up, upt = psf()
                    if cidx < 3:
                        mm(ops_[r0:r1, h * 128:(h + 1) * 128], hqtT[:, h, r0:r1], hsb[:, h, :], False, h == HH - 1 and cidx == nch - 1,
                           [t_hqtT, t_hsb[h]], [opt])
                        mm(up[:, 0:128], hkh[r0:r1, h * 128:(h + 1) * 128], hv[r0:r1, h * 128:(h + 1) * 128], True, True, [t_hkh, t_hv], [upt])
                    else:
                        mm(ops_[64:128, h * 128:(h + 1) * 128], hqtT3[:, h, :], hsb[:, h, :], False, h == HH - 1 and cidx == nch - 1,
                           [t_hqtT3, t_hsb[h]], [opt])
                        mm(up[:, 0:128], hkh3[64:128, h * 128:(h + 1) * 128], hv[64:128, h * 128:(h + 1) * 128], True, True, [t_hkh3, t_hv], [upt])
                    stt(hst[:, h, :], hst[:, h, :], dcs[:, h * 4 + cidx:h * 4 + cidx + 1], up[:, 0:128], ALU.mult, ALU.add,
                        [t_hst[h], t_dcs, upt], [t_hst[h]])
            act(sq[0:nq, 0:512], ops_[0:nq, :], AF.Square, [opt], [t_sq])
            P.emit("dve", lambda e: e.tensor_reduce(st1[0:nq, 8:12], hv3(sq[0:nq, 0:512], HH), AX.X, ALU.add), [t_sq], [t_st1])
            act(st1[0:nq, 8:12], st1[0:nq, 8:12], AF.Ln, [t_st1, t_eps], [t_st1], scale=1.0 / HK, bias=epsb[0:nq, 0:1])
            act(st1[0:nq, 8:12], st1[0:nq, 8:12], AF.Exp, [t_st1], [t_st1], scale=-0.5)
            tt("dve", hv3(W[0][0:nq, :], HH), hv3(ops_[0:nq, :], HH), st1[0:nq, 8:12].unsqueeze(2).to_broadcast([nq, HH, HK]), ALU.mult,
               [opt, t_st1], [tW[0]])
            tt("dve", omix[0:nq, 512:1024], W[0][0:nq, :], W[4][0:nq, :], ALU.mult, [tW[0], tW[4]], [t_omix])

        def project(nq, x_src):
            dma("sp", xt[0:nq, :], x_src, [], [t_xt])
            act(junk[0:nq, :], xt[0:nq, :], AF.Square, [t_xt], [t_junk, t_st1], accum=st1[0:nq, 15:16])
            act(st1[0:nq, 15:16], st1[0:nq, 15:16], AF.Ln, [t_st1, t_eps], [t_st1], scale=1.0 / D, bias=epsb[0:nq, 0:1])
            act(st1[0:nq, 15:16], st1[0:nq, 15:16], AF.Exp, [t_st1], [t_st1], scale=-0.5)
            stt(hn[0:nq, :], xt[0:nq, :], st1[0:nq, 15:16], gnorm[0:nq, :], ALU.mult, ALU.mult, [t_xt, t_st1, t_gn], [t_hn])
            pb, pbt = psb()
            for k in range(KC):
                tr(pb[:, k * 128:k * 128 + nq], hn[0:nq, k * 128:(k + 1) * 128], C["identb"][0:nq, 0:nq], [t_hn, CT["identb"]], [pbt])
            cp("dve", hT[:, :, 0:nq], pb[:].rearrange("p (k i) -> p k i", k=KC)[:, :, 0:nq], [pbt], [t_hT])

        def zc_mm(nq):
            def zc(c0, wd):
                z, zt = psf()
                for k in range(KC):
                    mm(z[0:nq, 0:wd], hT[:, k, 0:nq], win_sb[:, k, c0:c0 + wd], k == 0, k == KC - 1, [t_hT, t_win], [zt])
                return z[0:nq, 0:wd], [zt]
            return zc

        def mixer_tile(t, nq, zc, kv_dst, win_dst, NB, ncmp_valid, x1_dst, s=None):
            zA, zAt = zc(0, 512)
            norm_heads(zA, zAt, nq, NH, HD, qgs[0:nq, :], t_qg, qn[0:nq, :], t_qn, 0)
            pb, pbt = psb()
            for h in range(NH):
                tr(pb[0:64, h * 128:h * 128 + nq], qn[0:nq, h * 64:(h + 1) * 64], C["identb"][0:nq, 0:nq], [t_qn, CT["identb"]], [pbt])
            cp("act", qT[0:64, :, 0:nq], pb[0:64, :].rearrange("p (h i) -> p h i", h=NH)[:, :, 0:nq], [pbt], [t_qT])
            ts("dve", qT[64:KA, :, 0:nq], C["qabase"][64:KA, :, 0:nq], C["tcol"][64:KA, t:t + 1], None, ALU.mult, None,
               [CT["qabase"], CT["tcol"]], [t_qT])
            zB, zBt = zc(512, 512)
            cp("act", rows_sb[0:nq, 0:256], zB[:, 0:256], zBt, [t_rows])
            cp("act", rows_sb[0:nq, 384:512], zB[:, 384:512], zBt, [t_rows])
            norm_heads(zB[:, 256:384], zBt, nq, G, HD, kgs[0:nq, 64:128], t_kg, rows_sb[0:nq, 256:384], t_rows, 12)
            dma("sp", kv_dst, rows_sb[0:nq, :], [t_rows], [])
            cp("dve", kvb[0:nq, 0:512], rows_sb[0:nq, :], [t_rows], [t_kvb])
            zC, zCt = zc(1024, 280)
            norm_heads(zC[:, 0:128], zCt, nq, G, HD, kgs[0:nq, 128:192], t_kg, wrow_sb[0:nq, 0:128], t_wrow, 12)
            cp("act", wrow_sb[0:nq, 128:256], zC[:, 128:256], zCt, [t_wrow])
            if win_dst is not None:
                dma("sp", win_dst, wrow_sb[0:nq, 0:256], [t_wrow], [])
            cp("dve", kvb[0:nq, 512:768], wrow_sb[0:nq, 0:256], [t_wrow], [t_kvb])
            act(gates[0:nq, :], zC[:, 256:280], AF.Exp, zCt, [t_gates], scale=-1.0)
            ts("dve", gates[0:nq, :], gates[0:nq, :], 1.0, None, ALU.add, None, [t_gates], [t_gates])
            recip(gates[0:nq, :], gates[0:nq, :], [t_gates], [t_gates])
            sl = t % 8
            for g in range(G):
                pb, pbt = psb()
                tr(pb[0:64, 0:nq], kvb[0:nq, 512 + 64 * g:512 + 64 * g + 64], C["identb"][0:nq, 0:nq], [t_kvb, CT["identb"]], [pbt])
                if nq < 128:
                    memset("pool", kwinT[g][0:64, sl, :], 0.0, [t_winst[sl]])
                cp("act", kwinT[g][0:64, sl, 0:nq], pb[0:64, 0:nq], [pbt], [t_winst[sl]])
                memset("pool", kwinT[g][64:65, sl, :], float(t), [t_winst[sl]])
                if not sample:
                    tr(pb[0:64, 128:128 + nq], kvb[0:nq, 256 + 64 * g:256 + 64 * g + 64], C["identb"][0:nq, 0:nq], [t_kvb, CT["identb"]], [pbt])
                    cp("dve", kselT[g][0:64, 128 * t:128 * t + nq], pb[0:64, 128:128 + nq], [pbt], [t_ksel[g][t]])
            if nq < 128:
                memset("pool", vwin[:, sl, :, 0:64], 0.0, [t_winst[sl]])
            cp("pool", vwin[0:nq, sl, :, 0:64], kvb[0:nq, 640:768].rearrange("p (g d) -> p g d", g=G), [t_kvb], [t_winst[sl]])
            if not sample:
                cp("pool", vsel[0:nq, t, :, 0:64], kvb[0:nq, 384:512].rearrange("p (g d) -> p g d", g=G), [t_kvb], [t_vsel[t]])
                pb, pbt = psb()
                tr(pb[:, 0:nq], kvb[0:nq, 0:128], C["identb"][0:nq, 0:nq], [t_kvb, CT["identb"]], [pbt])
                tr(pb[:, 128:128 + nq], kvb[0:nq, 128:256], C["identb"][0:nq, 0:nq], [t_kvb, CT["identb"]], [pbt])
                cp("act", kcmpT[:, 128 * t:128 * t + nq], pb[:, 0:nq], [pbt], [t_kcmp[t]])
                cp("dve", vcmpT[:, 128 * t:128 * t + nq], pb[:, 128:128 + nq], [pbt], [t_kcmp[t]])
                nmax = min(8 * t + 6, ncmp_valid - 1)
                for v in range(NVT):
                    if 128 * v <= nmax and 128 * v + 127 >= 8 * t - 1:
                        M = min(128, nmax - 128 * v + 1)
                        ktoks = [t_kcmp[u] for u in range(16 * v, min(t, 16 * v + 16) + 1)]
                        compress_ntile(v, M, ktoks)
            for g in range(G):
                cmp_topk(g, t, nq, NB, ncmp_valid)
                if not sample:
                    sel_store(g, t, nq)
            if sample:
                sel_stream(l, s, nq)
            for g in range(G):
                win_branch(g, t, nq)
            cp("dve", omix[0:nq, 0:512], o_nsa[0:nq, :, :].rearrange("p h d -> p (h d)"), [t_onsa], [t_omix])
            hgrn_tile(nq, zc)
            pb, pbt = psb()
            for k in range(KC):
                tr(pb[:, k * 128:k * 128 + nq], omix[0:nq, k * 128:(k + 1) * 128], C["identb"][0:nq, 0:nq], [t_omix, CT["identb"]], [pbt])
            cp("act", oT[:, :, 0:nq], pb[:].rearrange("p (k i) -> p k i", k=KC)[:, :, 0:nq], [pbt], [t_oT])
            for half in range(2):
                y, yt = psf()
                for k in range(KC):
                    mm(y[0:nq, :], oT[:, k, 0:nq], wout_sb[:, k, half * 512:(half + 1) * 512], k == 0, k == KC - 1, [t_oT, t_wout], [yt])
                tt("dve", x1t[0:nq, half * 512:(half + 1) * 512], y[0:nq, :], xt[0:nq, half * 512:(half + 1) * 512], ALU.add, [yt, t_xt], [t_x1t])
            dma("sp", x1_dst, x1t[0:nq, :], [t_x1t], [])

        xin_p = xp_in if l == 0 else x2buf[0:S, :]
        xin_s = xs_in if l == 0 else x2buf[S:ROWS, :]
        if not sample:
            init_stores()
            memset("pool", hst[:], 0.0, t_hst)
            for t in range(NT if tiles_limit is None else tiles_limit):
                wd_ = None
                if 128 * t >= S - WIN:
                    w0 = 128 * t - (S - WIN)
                    wd_ = win_p[l, w0:w0 + 128, :]
                project(128, xin_p[128 * t:128 * t + 128, :])
                mixer_tile(t, 128, zc_mm(128), kv_p[l, 128 * t:128 * t + 128, :], wd_, c["NBP"], c["NCP"], x1buf[128 * t:128 * t + 128, :])
            for h in range(HH):
                dma("sp", hg_p[l, h], hst[:, h, :], [t_hst[h]], [])
            project(NSR, xin_s[0:NSR, :])
            zcs = zc_mm(NSR)
            for c0 in range(0, IN_COLS, 512):
                wd = min(512, IN_COLS - c0)
                z, zt = zcs(c0, wd)
                cp("act", tmpA[0:NSR, 0:wd], z, zt, [t_tmpA])
                dma("sp", zst[:, c0:c0 + wd], tmpA[0:NSR, 0:wd], [t_tmpA], [])
        else:
            for s in range(NSEQ):
                init_stores()
                for pg in range(NPAGE):
                    i2 = pg % 2; ip = pg % 4; ib = pg % 2
                    col = s * NPAGE + pg
                    gather(page[ip][:], cache[:, :], pidx_v[l][0][:, col:col + 1], [t_pidx], [t_page[ip]])
                    cp("dve", pageb[ib][:], page[ip][:], [t_page[ip]], [t_pageb[ib]])
                    pb, pbt = psb()
                    tr(pb[:, 0:128], pageb[ib][:, 0:128], C["identb"][:, :], [t_pageb[ib], CT["identb"]], [pbt])
                    tr(pb[:, 128:256], pageb[ib][:, 128:256], C["identb"][:, :], [t_pageb[ib], CT["identb"]], [pbt])
                    cp("act", kcmpT[:, 128 * pg:128 * pg + 128], pb[:, 0:128], [pbt], [t_kcmp[pg]])
                    cp("dve", vcmpT[:, 128 * pg:128 * pg + 128], pb[:, 128:256], [pbt], [t_kcmp[pg]])
                ncs = c["NCS"]
                for v in range((ncs + 127) // 128):
                    M = min(128, ncs - 128 * v)
                    ktoks = [t_kcmp[u] for u in range(16 * v, min(NPAGE - 1, 16 * v + 16) + 1)]
                    compress_ntile(v, M, ktoks)
                for wt in range(NWT):
                    u = TS - NWT + wt
                    i2 = wt % 2; ip = wt % 4; ib = wt % 2
                    dma("sp", page[ip][:], swin[l, s, wt * 128:(wt + 1) * 128, :], [], [t_page[ip]])
                    cp("dve", pageb[ib][:], page[ip][:], [t_page[ip]], [t_pageb[ib]])
                    for g in range(G):
                        pb, pbt = psb()
                        tr(pb[0:64, 0:128], pageb[ib][:, 64 * g:64 * g + 64], C["identb"][:, :], [t_pageb[ib], CT["identb"]], [pbt])
                        cp("act", kwinT[g][0:64, u % 8, :], pb[0:64, 0:128], [pbt], [t_winst[u % 8]])
                        memset("pool", kwinT[g][64:65, u % 8, :], float(u), [t_winst[u % 8]])
                    cp("pool", vwin[:, u % 8, :, 0:64], pageb[ib][:, 128:256].rearrange("p (g d) -> p g d", g=G), [t_pageb[ib]], [t_winst[u % 8]])
                for h in range(HH):
                    dma("sp", hst[:, h, :], shg[l, s, h], [], [t_hst[h]])
                dma("sp", zs[0:DS, :], zst[DS * s:DS * s + DS, :], [], [t_zs])
                dma("sp", xt[0:DS, :], xin_s[DS * s:DS * s + DS, :], [], [t_xt])

                def zc_s(c0, wd):
                    return zs[0:DS, c0:c0 + wd], [t_zs]
                mixer_tile(TS, DS, zc_s, kv_s[l, DS * s:DS * s + DS, :], win_s[l, s, WIN - DS:WIN, :], c["NBS"], c["NCS"],
                           x1buf[S + DS * s:S + DS * s + DS, :], s=s)
                dma("sp", win_s[l, s, 0:WIN - DS, :], swin[l, s, DS:WIN, :], [], [])
                for h in range(HH):
                    dma("sp", hg_s[l, s, h], hst[:, h, :], [t_hst[h]], [])
        P.flush()
        ps_.close()

    fcnt = {"n": 0}

    def ffn_phase(l, row_tiles, last, FG=4):
        fcnt["n"] += 1
        fid = fcnt["n"]
        ps_ = ExitStack()
        moe = (l == 1) and os.environ.get("MOEDBG") != "4"
        TOKB = 512
        NTI = len(row_tiles)
        NCOL = 128 * NTI

        def sbp(name, shape, dt=F32):
            return sbx(ps_, "f%d_%d_" % (l, fid) + name, shape, dt)
        hTf = sbp("hTf", [128, KC, NCOL], BF16); t_hTf = TK()
        yacc = sbp("yacc", [128, NTI, D]); t_yacc = TK(NTI)
        gate_sb = sbp("gate_sb", [128, NTI, 8]); t_gate = TK()
        hT32 = sbp("hT32", [128, KC, 128]); t_hT32 = TK()
        hn32 = sbp("hn32", [128, D]); t_hn32 = TK()
        rt_sb = sbp("rt_sb", [128, KC, 8]); t_rt = TK()
        wg_sb = [sbp("wg_sb%d" % i, [128, KC, FG * 128], BF16) for i in range(2)]
        wu_sb = [sbp("wu_sb%d" % i, [128, KC, FG * 128], BF16) for i in range(2)]
        wd_sb = [sbp("wd_sb%d" % i, [128, FG, D], BF16) for i in range(2)]
        t_wf = TK(2)
        actT = sbp("actT", [128, FG, NCOL], BF16); t_actT = TK()
        sgm2 = [sbp("sgm%d" % i_, [128, 512], BF16) for i_ in range(2)]; t_sgm2 = TK(2)
        scnt = {"n": 0}
        lg = sbp("lg", [128, 32]); t_lg = TK()
        P.barrier()
        rr["nrot"] = 4
        dma("sp", gnorm[:], nffn[l], [], [t_gn])
        DBG5 = os.environ.get("MOEDBG") == "5"
        if moe and not DBG5 and int(os.environ.get("RLVL", "9")) >= 1:
            for k in range(KC):
                dma("sp", rt_sb[:, k, :], rtr[k * 128:(k + 1) * 128, :], [], [t_rt])
        col0 = [128 * i for i in range(NTI)]
        ncols = 128 * (NTI - 1) + row_tiles[-1][1]

        def src_of(r0, n):
            return x1buf[r0:r0 + n, :]

        def dst_of(r0, n):
            if r0 < 0:
                j = -r0 - 1
                return y_p[128 * j:128 * j + 128, :]
            if not last:
                return x2buf[r0:r0 + n, :]
            return y_s[r0 - S:r0 - S + n, :]
        for i, (r0, n) in enumerate(row_tiles):
            if r0 < 0:
                j = -r0 - 1
                gather(xt[0:n, :], x1buf[:, :], ridx[:, j:j + 1], [t_ridx], [t_xt])
            else:
                dma("sp", xt[0:n, :], src_of(r0, n), [], [t_xt])
            act(junk[0:n, :], xt[0:n, :], AF.Square, [t_xt], [t_junk, t_st1], accum=st1[0:n, 15:16])
            act(st1[0:n, 15:16], st1[0:n, 15:16], AF.Ln, [t_st1, t_eps], [t_st1], scale=1.0 / D, bias=epsb[0:n, 0:1])
            act(st1[0:n, 15:16], st1[0:n, 15:16], AF.Exp, [t_st1], [t_st1], scale=-0.5)
            stt(hn32[0:n, :], xt[0:n, :], st1[0:n, 15:16], gnorm[0:n, :], ALU.mult, ALU.mult, [t_xt, t_st1, t_gn], [t_hn32])
            cp("pool", yacc[0:n, i, :], xt[0:n, :], [t_xt], [t_yacc[i]])
            for half in range(2):
                pf, pft = psf()
                for k in range(4):
                    kk = half * 4 + k
                    tr(pf[:, k * 128:k * 128 + n], hn32[0:n, kk * 128:(kk + 1) * 128], C["identf"][0:n, 0:n], [t_hn32, CT["identf"]], [pft])
                v4 = pf[:].rearrange("p (k i) -> p k i", k=4)
                cp("act", hTf[:, half * 4:half * 4 + 4, col0[i]:col0[i] + n], v4[:, :, 0:n], [pft], [t_hTf])
                if moe and not DBG5 and int(os.environ.get("RLVL", "9")) >= 2:
                    cp("act", hT32[:, half * 4:half * 4 + 4, 0:n], v4[:, :, 0:n], [pft], [t_hT32])
            RL = int(os.environ.get("RLVL", "9"))
            if moe and (os.environ.get("MOEDBG") in ("1", "3", "5") or RL < 6):
                memset("dve", gate_sb[0:n, i, :], 0.25, t_gate)
            if moe and os.environ.get("MOEDBG") not in ("1", "3", "5"):
                if RL >= 3:
                    lp, lpt = psf()
                    for k in range(KC):
                        mm(lp[0:n, 0:8], hT32[:, k, 0:n], rt_sb[:, k, :], k == 0, k == KC - 1, [t_hT32, t_rt], [lpt])
                    cp("act", lg[0:n, 0:8], lp[0:n, 0:8], [lpt], [t_lg])
                if RL >= 4:
                    P.emit("dve", lambda e, n=n: e.max(lg[0:n, 8:16], lg[0:n, 0:8]), [t_lg], [t_lg])
                if RL >= 5:
                    tt("dve", lg[0:n, 16:17], lg[0:n, 9:10], lg[0:n, 8:9], ALU.subtract, [t_lg], [t_lg])
                    act(lg[0:n, 16:17], lg[0:n, 16:17], AF.Exp, [t_lg], [t_lg])
                    ts("dve", lg[0:n, 16:17], lg[0:n, 16:17], 1.0, None, ALU.add, None, [t_lg], [t_lg])
                    recip(lg[0:n, 17:18], lg[0:n, 16:17], [t_lg], [t_lg])
                    ts("dve", lg[0:n, 18:19], lg[0:n, 17:18], -1.0, 1.0, ALU.mult, ALU.add, [t_lg], [t_lg])
                if RL >= 6:
                    ts("dve", lg[0:n, 20:28], lg[0:n, 0:8], lg[0:n, 8:9], lg[0:n, 17:18], ALU.is_equal, ALU.mult, [t_lg], [t_lg])
                    ts("dve", gate_sb[0:n, i, :], lg[0:n, 0:8], lg[0:n, 9:10], lg[0:n, 18:19], ALU.is_equal, ALU.mult, [t_lg], [t_gate])
                    tt("dve", gate_sb[0:n, i, :], gate_sb[0:n, i, :], lg[0:n, 20:28], ALU.add, [t_gate, t_lg], [t_gate])
        nfg = (NFC + FG - 1) // FG
        wi = 0
        experts = list(range(NE)) if moe else [0]
        if moe and os.environ.get("MOEDBG") in ("2", "5"):
            experts = [int(x) for x in os.environ.get("EXPL", "0").split(",")]
        for e_ in experts:
            wgd = mwg[e_] if moe else fwg; wud = mwu[e_] if moe else fwu; wdd = mwd[e_] if moe else fwd
            wgv = wgd.rearrange("(k p) f -> p k f", p=128); wuv = wud.rearrange("(k p) f -> p k f", p=128)
            wdv = wdd.rearrange("(c p) d -> p c d", p=128)
            for fg in range(nfg):
                f0 = fg * FG; nf = min(FG, NFC - f0)
                b = wi % 2; wi += 1
                for k in range(KC):
                    dma("pool", wg_sb[b][:, k, 0:nf * 128], wgv[:, k, f0 * 128:(f0 + nf) * 128], [], [t_wf[b]])
                    dma("pool", wu_sb[b][:, k, 0:nf * 128], wuv[:, k, f0 * 128:(f0 + nf) * 128], [], [t_wf[b]])
                for cc in range(nf):
                    dma("pool", wd_sb[b][:, cc, :], wdv[:, f0 + cc, :], [], [t_wf[b]])
                for tb0 in range(0, ncols, TOKB):
                    tw = min(TOKB, ncols - tb0)
                    for cc in range(nf):
                        pa, pat = psf(); pu, put = psf()
                        for k in range(KC):
                            mm(pa[:, 0:tw], wg_sb[b][:, k, cc * 128:(cc + 1) * 128], hTf[:, k, tb0:tb0 + tw], k == 0, k == KC - 1, [t_wf[b], t_hTf], [pat])
                        for k in range(KC):
                            mm(pu[:, 0:tw], wu_sb[b][:, k, cc * 128:(cc + 1) * 128], hTf[:, k, tb0:tb0 + tw], k == 0, k == KC - 1, [t_wf[b], t_hTf], [put])
                        si = scnt["n"] % 2; scnt["n"] += 1
                        act(sgm2[si][:, 0:tw], pa[:, 0:tw], AF.Silu, [pat], [t_sgm2[si]])
                        tt("dve", actT[:, cc, tb0:tb0 + tw], pu[:, 0:tw], sgm2[si][:, 0:tw], ALU.mult, [put, t_sgm2[si]], [t_actT])
                    for i in range(tb0 // 128, (tb0 + tw + 127) // 128):
                        n = row_tiles[i][1]
                        for half in range(2):
                            py, pyt = PSF[4 + half], tPSF[4 + half]
                            for cc in range(nf):
                                mm(py[0:n, :], actT[:, cc, col0[i]:col0[i] + n], wd_sb[b][:, cc, half * 512:(half + 1) * 512], cc == 0, cc == nf - 1,
                                   [t_actT, t_wf[b]], [pyt])
                            ysl = yacc[0:n, i, half * 512:(half + 1) * 512]
                            if moe and not DBG5:
                                stt(ysl, py[0:n, :], gate_sb[0:n, i, e_:e_ + 1], ysl, ALU.mult, ALU.add, [pyt, t_gate, t_yacc[i]], [t_yacc[i]])
                            else:
                                tt("dve", ysl, py[0:n, :], ysl, ALU.add, [pyt, t_yacc[i]], [t_yacc[i]])
        for i, (r0, n) in enumerate(row_tiles):
            dma("sp", dst_of(r0, n), yacc[0:n, i, :], [t_yacc[i]], [])
        P.flush()
        ps_.close()

    dma("sp", pidx[:], ptab[:], [], [t_pidx])
    dma("sp", ridx[:], ridx_in[:], [], [t_ridx])
    P.emit("pool", lambda e: e.iota(iota_p[:], [[0, 1]], base=0, channel_multiplier=1, allow_small_or_imprecise_dtypes=True), (), [t_iota])
    cp("dve", pidxf[:], pidx[:], [t_pidx], [t_pidxf])
    ts("dve", pidxf[:], pidxf[:], 128.0, iota_p[:, 0:1], ALU.mult, ALU.add, [t_pidxf, t_iota], [t_pidxf])
    for l_ in range(2):
        for h_ in range(2):
            ts("dve", W0f[:], pidxf[:], 2.0, float(2 * l_ * NPOOL * 128 + h_), ALU.mult, ALU.add, [t_pidxf], [t_W0f])
            cp("dve", pidx_v[l_][h_][:], W0f[:], [t_W0f], [t_pidx])

    tiles = [(128 * i, 128) for i in range(NT)] + [(S, NSR)]
    nph = 0
    for l in range(2):
        for ph in ("p", "s", "f"):
            if nph >= stop_after:
                break
            nph += 1
            if ph == "p":
                mixer_phase(l, False)
            elif ph == "s":
                mixer_phase(l, True)
            else:
                FG_ = int(os.environ.get("FFNG", "9"))
                tl = tiles if l == 0 else ([(-(j + 1), 128) for j in range(NTH)] + [(S, NSR)])
                if l == 1 and "FFNG1" in os.environ:
                    ffn_phase(l, tl, True, FG=2)
                else:
                    for g0 in range(0, len(tl), FG_):
                        ffn_phase(l, tl[g0:g0 + FG_], l == 1)
    P.barrier()
    P.flush(final=True)
    es.close()
    return nc, consts, P.ninst


def make_in_maps(cfg, inputs, consts, ncores=8):
    c = derive(cfg)
    S, NSEQ, DS, NPAGE, WIN = c["S"], c["NSEQ"], c["DS"], c["NPAGE"], c["WIN"]
    f = lambda a: np.ascontiguousarray(np.asarray(a), dtype=np.float32)
    B = inputs["x_prompt"].shape[0]
    cache = f(inputs["cache_kv"]).reshape(-1, 256)
    rep = lambda a, tail: np.ascontiguousarray(np.broadcast_to(f(a).reshape(2, 1, tail), (2, 128, tail)))
    shared = {
        "cache": cache,
        "nmix": rep(inputs["norm_mix"], D), "nffn": rep(inputs["norm_ffn"], D),
        "w_in": f(inputs["w_in"]), "w_out": f(inputs["w_out"]),
        "qg": rep(inputs["q_gain"], HD), "kg": rep(inputs["k_gain"], 3 * HD),
        "cpe": f(inputs["cmp_pe"]), "cw": f(inputs["cmp_w"]),
        "lbl": np.ascontiguousarray(np.broadcast_to(f(inputs["hgrn_lb_logits"]).reshape(1, 2, HH * HK), (128, 2, HH * HK))),
        "ogn": rep(inputs["hgrn_o_gain"], HK),
        "fwg": f(inputs["ffn_w_gate"])[0], "fwu": f(inputs["ffn_w_up"])[0], "fwd": f(inputs["ffn_w_down"])[0],
        "rtr": f(inputs["moe_router"])[0], "mwg": f(inputs["moe_w_gate"])[0], "mwu": f(inputs["moe_w_up"])[0], "mwd": f(inputs["moe_w_down"])[0],
    }
    for k, v in consts.items():
        shared["c_" + k] = v
    maps = []
    pt = np.asarray(inputs["page_table"]).astype(np.int32)
    for core in range(ncores):
        b = core % B
        s0 = core * NSEQ
        m = dict(shared)
        m["xp"] = f(inputs["x_prompt"][b])
        m["xs"] = f(inputs["x_sample"][s0:s0 + NSEQ]).reshape(NSEQ * DS, D)
        m["swin"] = f(inputs["state_win_kv"][:, s0:s0 + NSEQ]).reshape(2, NSEQ, WIN, 256)
        m["shg"] = f(inputs["state_hgrn"][:, s0:s0 + NSEQ])
        m["ptab"] = np.ascontiguousarray(np.broadcast_to(pt[s0:s0 + NSEQ].reshape(1, NSEQ * NPAGE), (128, NSEQ * NPAGE)))
        hh = core // B
        nth = (S // 128) // 2
        m["ridx"] = ((hh * nth + np.arange(nth)[None, :]) * 128 + np.arange(128)[:, None]).astype(np.int32)
        maps.append(m)
    return maps


def assemble(cfg, r, B, ncores=8):
    c = derive(cfg)
    S, NSEQ, DS, WIN = c["S"], c["NSEQ"], c["DS"], c["WIN"]
    y_p = np.stack([np.concatenate([r[b]["y_p"], r[b + B]["y_p"]], axis=0) for b in range(B)])
    y_s = np.concatenate([r[k]["y_s"].reshape(NSEQ, DS, D) for k in range(ncores)])
    kv_p = np.stack([r[b]["kv_p"] for b in range(B)], axis=1).reshape(2, B, S, 4, G, HD)
    kv_s = np.concatenate([r[k]["kv_s"].reshape(2, NSEQ, DS, 4, G, HD) for k in range(ncores)], axis=1)
    win_p = np.stack([r[b]["win_p"] for b in range(B)], axis=1).reshape(2, B, WIN, 2, G, HD)
    win_s = np.concatenate([r[k]["win_s"].reshape(2, NSEQ, WIN, 2, G, HD) for k in range(ncores)], axis=1)
    hg_p = np.stack([r[b]["hg_p"] for b in range(B)], axis=1)
    hg_s = np.concatenate([r[k]["hg_s"] for k in range(ncores)], axis=1)
    return tuple(np.ascontiguousarray(a, dtype=np.float32) for a in (y_p, y_s, kv_p, kv_s, win_p, win_s, hg_p, hg_s))


_CACHE = {}


def kernel(**inputs):
    cfg = full_cfg()
    if "nc" not in _CACHE:
        _CACHE["nc"] = build_program(cfg)
    nc, consts, _ = _CACHE["nc"]
    maps = make_in_maps(cfg, inputs, consts)
    res = run_bass_kernel_spmd(nc, maps, core_ids=list(range(8)))
    return assemble(cfg, res.results, 4)
```
